# Optimizing a Trainium2 kernel written in Bass

```python
import jax
import jax.numpy as jnp
from jax import lax
import numpy as np

D_MODEL = 1024
BATCH = 16
SEQ = 2048
DEPTH = 2

N_BRANCH = 4
HEADS = 4
BRANCH_WIDTH = D_MODEL // 4
HEAD_DV = BRANCH_WIDTH // HEADS
RET_DK = HEAD_DV // 2
GLA_DK = HEAD_DV // 2
HGRN_DK = HEAD_DV
RWKV_N = HEAD_DV
RWKV_W_LORA = 64
RWKV_A_LORA = 64
RWKV_G_LORA = 128
GLA_GATE_LORA = 16
GLA_GATE_TEMP = 16.0
CHUNK = 64
RET_ROPE_BASE = 10000.0
RWKV_DECAY_SCALE = 0.6065306597126334
D_FF = 2816
FFN_CONV_WIDTH = 3
PLE_DIM = 256
LN_EPS = 1e-5
HEAD_EPS = 1e-6
RWKV_GN_EPS = 64e-5
ALPHA = (2.0 * DEPTH) ** 0.25
BETA = (8.0 * DEPTH) ** -0.25

RET_COLS = (HEADS * RET_DK, HEADS * RET_DK, BRANCH_WIDTH, BRANCH_WIDTH)
RWKV_COLS = (BRANCH_WIDTH, BRANCH_WIDTH, BRANCH_WIDTH, RWKV_W_LORA, RWKV_A_LORA, RWKV_G_LORA)
GLA_COLS = (HEADS * GLA_DK, HEADS * GLA_DK, BRANCH_WIDTH, GLA_GATE_LORA, BRANCH_WIDTH)
HGRN_COLS = (HEADS * HGRN_DK, HEADS * HGRN_DK, BRANCH_WIDTH, BRANCH_WIDTH)
GROUP_COLS = (sum(RET_COLS), sum(RWKV_COLS), sum(GLA_COLS), sum(HGRN_COLS), N_BRANCH * D_MODEL)
N_IN = sum(GROUP_COLS)

kernel_name = "hybrid_gated_retnet_rwkv7_gla_hgrn2"


def split_cols(a, sizes):
    return jnp.split(a, [int(c) for c in np.cumsum(sizes)[:-1]], axis=-1)


def heads(a, n_heads=HEADS):
    return a.reshape(*a.shape[:-1], n_heads, a.shape[-1] // n_heads)


def bhsd(a):
    return a.transpose(0, 2, 1, 3)


def shift_right(a):
    return jnp.pad(a, ((0, 0), (1, 0), (0, 0)))[:, :-1]


def layer_norm(x, w, b):
    x32 = x.astype(jnp.float32)
    mu = jnp.mean(x32, -1, keepdims=True)
    var = jnp.mean(jnp.square(x32 - mu), -1, keepdims=True)
    return ((x32 - mu) * lax.rsqrt(var + LN_EPS) * w + b).astype(x.dtype)


def head_norm(y, eps):
    y = y.astype(jnp.float32)
    mu = jnp.mean(y, -1, keepdims=True)
    var = jnp.mean(jnp.square(y - mu), -1, keepdims=True)
    return (y - mu) * lax.rsqrt(var + eps)


def head_rms(y, gain):
    y = y.astype(jnp.float32)
    return y * lax.rsqrt(jnp.mean(jnp.square(y), -1, keepdims=True) + HEAD_EPS) * gain


def rotary(x):
    S, d = x.shape[1], x.shape[-1]
    theta = 1.0 / (RET_ROPE_BASE ** jnp.linspace(0.0, 1.0, d // 2))
    ang = jnp.arange(S, dtype=jnp.float32)[:, None] * theta[None, :]
    cos, sin = jnp.cos(ang)[:, None, :], jnp.sin(ang)[:, None, :]
    xr = x.astype(jnp.float32).reshape(*x.shape[:-1], d // 2, 2)
    x1, x2 = xr[..., 0], xr[..., 1]
    return jnp.stack([x1 * cos - x2 * sin, x1 * sin + x2 * cos], -1).reshape(x.shape)


def to_chunks(a):
    B, H, T, d = a.shape
    return a.reshape(B, H, T // CHUNK, CHUNK, d).transpose(2, 0, 1, 3, 4)


def from_chunks(o):
    n, B, H, C, d = o.shape
    return o.transpose(1, 2, 0, 3, 4).reshape(B, H, n * C, d)


def retention_chunkwise(q, k, v, log_gamma):
    B, H, T, dk = q.shape
    dv = v.shape[-1]
    pos = jnp.arange(CHUNK, dtype=jnp.float32)
    causal = pos[:, None] >= pos[None, :]
    rel = jnp.where(causal, pos[:, None] - pos[None, :], 0.0)
    decay = jnp.where(causal[None], jnp.exp(log_gamma[:, None, None] * rel[None]), 0.0)
    xi = jnp.exp(log_gamma[:, None] * (pos + 1.0))[None, :, :, None]
    zeta = jnp.exp(log_gamma[:, None] * (CHUNK - 1.0 - pos))[None, :, :, None]
    gamma_c = jnp.exp(log_gamma * CHUNK)[None, :, None, None]

    def step(R, inp):
        qc, kc, vc = inp
        scores = jnp.einsum('bhtd,bhsd->bhts', qc, kc) * decay[None]
        o = jnp.einsum('bhts,bhse->bhte', scores, vc) + jnp.einsum('bhtd,bhde->bhte', qc, R) * xi
        R = gamma_c * R + jnp.einsum('bhsd,bhse->bhde', kc, vc * zeta)
        return R, o

    R0 = jnp.zeros((B, H, dk, dv), jnp.float32)
    _, o = lax.scan(step, R0, (to_chunks(q), to_chunks(k), to_chunks(v)))
    return from_chunks(o)


def chunk_gated_linear_attention(q, k, v, log_g):
    B, H, T, dk = q.shape
    dv = v.shape[-1]
    causal = jnp.tril(jnp.ones((CHUNK, CHUNK), bool))[:, :, None]

    def step(S, inp):
        qc, kc, vc, gc = inp
        b = jnp.cumsum(gc, axis=2)
        diff = b[:, :, :, None, :] - b[:, :, None, :, :]
        decay = jnp.where(causal, jnp.exp(jnp.where(causal, diff, 0.0)), 0.0)
        scores = jnp.einsum('bhtd,bhsd,bhtsd->bhts', qc, kc, decay)
        o = jnp.einsum('bhts,bhse->bhte', scores, vc) + jnp.einsum('bhtd,bhde->bhte', qc * jnp.exp(b), S)
        b_last = b[:, :, -1:, :]
        S = jnp.exp(b_last[:, :, 0, :, None]) * S + jnp.einsum('bhsd,bhse->bhde', kc * jnp.exp(b_last - b), vc)
        return S, o

    S0 = jnp.zeros((B, H, dk, dv), jnp.float32)
    _, o = lax.scan(step, S0, (to_chunks(q), to_chunks(k), to_chunks(v), to_chunks(log_g)))
    return from_chunks(o)


def rwkv7_recurrence(r, w, k, v, kk, a):
    B, S, H, N = r.shape
    seq = tuple(t.transpose(1, 0, 2, 3) for t in (r, w, k, v, kk, a))

    def step(state, inp):
        rt, wt, kt, vt, kkt, at = inp
        sa = jnp.einsum('bhvk,bhk->bhv', state, kkt)
        state = state * wt[:, :, None, :] - sa[..., None] * (kkt * at)[:, :, None, :] + vt[..., None] * kt[:, :, None, :]
        return state, jnp.einsum('bhvk,bhk->bhv', state, rt)

    _, y = lax.scan(step, jnp.zeros((B, H, N, N), jnp.float32), seq)
    return y.transpose(1, 0, 2, 3)


def retnet_branch(z):
    B, S, _ = z.shape
    q, k, v, g = split_cols(z.astype(jnp.float32), RET_COLS)
    q = rotary(heads(q))
    k = rotary(heads(k)) * RET_DK ** -0.5
    log_gamma = jnp.log1p(-jnp.exp2(-5.0 - jnp.arange(HEADS, dtype=jnp.float32)))
    o = retention_chunkwise(bhsd(q), bhsd(k), bhsd(heads(v)), log_gamma)
    o = head_norm(bhsd(o), HEAD_EPS).reshape(B, S, BRANCH_WIDTH)
    return (jax.nn.silu(g) * o).astype(z.dtype)


def rwkv7_branch(z, mu, w0, w2, a0, a2, g2, k_k, k_a, r_k, ln_w, ln_b):
    B, S, _ = z.shape
    z = z.astype(jnp.float32)
    z = z + (shift_right(z) - z) * mu
    r, k, v, wl, al, gl = split_cols(z, RWKV_COLS)
    w = jnp.exp(-RWKV_DECAY_SCALE * jax.nn.sigmoid(w0 + jnp.tanh(wl) @ w2))
    a = jax.nn.sigmoid(a0 + al @ a2)
    g = jax.nn.sigmoid(gl) @ g2
    kk = heads(k * k_k)
    kk = kk * lax.rsqrt(jnp.sum(jnp.square(kk), -1, keepdims=True) + 1e-12)
    k = k * (1.0 + (a - 1.0) * k_a)
    rh, kh, vh = heads(r), heads(k), heads(v)
    y = rwkv7_recurrence(rh, heads(w), kh, vh, kk, heads(a))
    y = head_norm(y, RWKV_GN_EPS).reshape(B, S, BRANCH_WIDTH) * ln_w + ln_b
    bonus = (jnp.sum(rh * kh * r_k, -1, keepdims=True) * vh).reshape(B, S, BRANCH_WIDTH)
    return ((y + bonus) * g).astype(z.dtype)


def gla_branch(z, w2, b, norm_w):
    B, S, _ = z.shape
    q, k, v, gl, g = split_cols(z.astype(jnp.float32), GLA_COLS)
    log_alpha = jax.nn.log_sigmoid(gl @ w2 + b) / GLA_GATE_TEMP
    o = chunk_gated_linear_attention(bhsd(heads(q) * GLA_DK ** -0.5), bhsd(heads(k)), bhsd(heads(v)), bhsd(heads(log_alpha)))
    o = head_rms(bhsd(o), norm_w).reshape(B, S, BRANCH_WIDTH)
    return (o * jax.nn.silu(g)).astype(z.dtype)


def hgrn2_branch(z, lb, norm_w):
    B, S, _ = z.shape
    q, fz, i, g = split_cols(z.astype(jnp.float32), HGRN_COLS)
    f = lb + (1.0 - lb) * jax.nn.sigmoid(fz)
    log_f = jnp.log(f)
    k = (1.0 - lb) * jax.nn.sigmoid(-fz)
    o = chunk_gated_linear_attention(bhsd(heads(q)), bhsd(heads(k)), bhsd(heads(i)), bhsd(heads(log_f)))
    o = head_rms(bhsd(o), norm_w).reshape(B, S, BRANCH_WIDTH)
    return (o * jax.nn.silu(g)).astype(z.dtype)


def conv_ffn(x, w_up, conv_w, w_down):
    S = x.shape[1]
    u, v = jnp.split(x @ w_up, 2, axis=-1)
    up = jnp.pad(u, ((0, 0), (FFN_CONV_WIDTH - 1, 0), (0, 0)))
    u = sum(conv_w[j] * up[:, j:j + S] for j in range(FFN_CONV_WIDTH))
    return (jax.nn.gelu(u) * v) @ w_down


def _normal(k, shape, scale):
    return scale * jax.random.normal(k, shape, jnp.float32)


def setup_inputs(seed: int = 0) -> dict:
    key = jax.random.key(seed)
    k = jax.random.split(key, 32)
    L, D, W = DEPTH, D_MODEL, BRANCH_WIDTH
    return {
        'x': _normal(k[0], (BATCH, SEQ, D), 1.0),
        'p': _normal(k[1], (DEPTH, BATCH, SEQ, PLE_DIM), 1.0),
        'ln_in_w': 1.0 + _normal(k[2], (D,), 0.02),
        'ln_in_b': _normal(k[3], (D,), 0.02),
        'w_in': _normal(k[4], (L, D, N_IN), D ** -0.5),
        'rwkv_mu': jax.random.uniform(k[5], (L, sum(RWKV_COLS)), jnp.float32),
        'rwkv_w0': _normal(k[6], (L, W), 0.5),
        'rwkv_w2': _normal(k[7], (L, RWKV_W_LORA, W), 0.5 * RWKV_W_LORA ** -0.5),
        'rwkv_a0': _normal(k[8], (L, W), 0.1),
        'rwkv_a2': _normal(k[9], (L, RWKV_A_LORA, W), 0.5 * RWKV_A_LORA ** -0.5),
        'rwkv_g2': _normal(k[10], (L, RWKV_G_LORA, W), RWKV_G_LORA ** -0.5),
        'rwkv_k_k': 0.85 + _normal(k[11], (L, W), 0.05),
        'rwkv_k_a': 1.0 + _normal(k[12], (L, W), 0.05),
        'rwkv_r_k': _normal(k[13], (L, HEADS, RWKV_N), 0.1),
        'rwkv_ln_w': 1.0 + _normal(k[14], (L, W), 0.02),
        'rwkv_ln_b': _normal(k[15], (L, W), 0.02),
        'gla_w2': _normal(k[16], (L, GLA_GATE_LORA, HEADS * GLA_DK), GLA_GATE_LORA ** -0.5),
        'gla_b': _normal(k[17], (L, HEADS * GLA_DK), 0.1),
        'gla_norm_w': 1.0 + _normal(k[18], (L, HEAD_DV), 0.02),
        'hgrn_lb_logits': _normal(k[19], (L, HEADS * HGRN_DK), 0.1),
        'hgrn_norm_w': 1.0 + _normal(k[20], (L, HEAD_DV), 0.02),
        'w_branch': _normal(k[21], (L, N_BRANCH, W, D), W ** -0.5),
        'w_mix_out': _normal(k[22], (L, D, D), BETA * D ** -0.5),
        'ln_mix_w': 1.0 + _normal(k[23], (L, D), 0.02),
        'ln_mix_b': _normal(k[24], (L, D), 0.02),
        'w_ffn_up': _normal(k[25], (L, D, 2 * D_FF), D ** -0.5),
        'ffn_conv': _normal(k[26], (L, FFN_CONV_WIDTH, D_FF), FFN_CONV_WIDTH ** -0.5),
        'w_ffn_down': _normal(k[27], (L, D_FF, D), BETA * D_FF ** -0.5),
        'w_ple_gate': _normal(k[28], (L, D, D), D ** -0.5),
        'w_ple_proj': _normal(k[29], (L, PLE_DIM, D), BETA * PLE_DIM ** -0.5),
        'ln_ffn_w': 1.0 + _normal(k[30], (L, D), 0.02),
        'ln_ffn_b': _normal(k[31], (L, D), 0.02),
    }


def reference(x, p, ln_in_w, ln_in_b, w_in, rwkv_mu, rwkv_w0, rwkv_w2, rwkv_a0, rwkv_a2, rwkv_g2,
              rwkv_k_k, rwkv_k_a, rwkv_r_k, rwkv_ln_w, rwkv_ln_b, gla_w2, gla_b, gla_norm_w,
              hgrn_lb_logits, hgrn_norm_w, w_branch, w_mix_out, ln_mix_w, ln_mix_b,
              w_ffn_up, ffn_conv, w_ffn_down, w_ple_gate, w_ple_proj, ln_ffn_w, ln_ffn_b):
    lb_prob = jax.nn.softmax(hgrn_lb_logits.astype(jnp.float32), axis=0)
    lb_all = jnp.concatenate([jnp.zeros_like(lb_prob[:1]), jnp.cumsum(lb_prob[1:], axis=0)], axis=0)

    x = layer_norm(x, ln_in_w, ln_in_b)
    for i in range(DEPTH):
        z = x @ w_in[i]
        z_ret, z_rwkv, z_gla, z_hgrn, z_gate = split_cols(z, GROUP_COLS)
        branches = (
            retnet_branch(z_ret),
            rwkv7_branch(z_rwkv, rwkv_mu[i], rwkv_w0[i], rwkv_w2[i], rwkv_a0[i], rwkv_a2[i], rwkv_g2[i],
                         rwkv_k_k[i], rwkv_k_a[i], rwkv_r_k[i], rwkv_ln_w[i], rwkv_ln_b[i]),
            gla_branch(z_gla, gla_w2[i], gla_b[i], gla_norm_w[i]),
            hgrn2_branch(z_hgrn, lb_all[i], hgrn_norm_w[i]),
        )
        merged = jnp.zeros_like(x)
        for n in range(N_BRANCH):
            gate = jax.nn.sigmoid(z_gate[..., n * D_MODEL:(n + 1) * D_MODEL])
            merged = merged + gate * (branches[n] @ w_branch[i, n])
        x = layer_norm(ALPHA * x + merged @ w_mix_out[i], ln_mix_w[i], ln_mix_b[i])
        ffn = conv_ffn(x, w_ffn_up[i], ffn_conv[i], w_ffn_down[i])
        ple = jax.nn.sigmoid(x @ w_ple_gate[i]) * (p[i] @ w_ple_proj[i])
        x = layer_norm(ALPHA * x + ffn + ple, ln_ffn_w[i], ln_ffn_b[i])
    return x
```

```python
import numpy as np
import concourse.bass as bass
import concourse.mybir as mybir
from concourse.bass_utils import run_bass_kernel_spmd

F32 = mybir.dt.float32
BF16 = mybir.dt.bfloat16
AF = mybir.ActivationFunctionType
ALU = mybir.AluOpType
AX = mybir.AxisListType

EPOCH = 30000
NRING = 8

D = 1024
DEPTH = 2
NIN = 7696
DFF = 2816
ALPHA = (2.0 * DEPTH) ** 0.25
OFF_RET, OFF_RWKV, OFF_GLA, OFF_HGRN, OFF_GATE = 0, 768, 1792, 2576, 3600
NMIX = 3600


class Tile:
    _n = 0

    def __init__(self, handle, name):
        self.h = handle
        self.name = name
        Tile._n += 1
        self.id = Tile._n

    def __getitem__(self, k):
        return self.h[k]

    def ap(self):
        return self.h.ap()

    def k(self, sub):
        return (self.id, sub)


def _key(k):
    return (k.id, None) if isinstance(k, Tile) else k


class Prog:
    STREAMS = ("pe", "act", "dve", "pool", "sp")

    def __init__(self, nc):
        self.nc = nc
        self.ops = {e: [] for e in self.STREAMS}
        self.cc = {e: 0 for e in self.STREAMS}
        self.cd = {e: 0 for e in self.STREAMS}
        self.state = {}
        self.waited = {e: {} for e in self.STREAMS}
        self.sems = {}
        self.rot = {}
        self.latest = {}
        self.scopes = []
        self.uid = 0

    def sb(self, name, shape, dtype=F32):
        self.uid += 1
        g = self.nc.sbuf_tensor(f"{name}_{self.uid}", list(shape), dtype)
        h = g.__enter__()
        if self.scopes:
            self.scopes[-1][0].append(g)
        return Tile(h, name)

    def push(self):
        self.scopes.append([[], [], {}])

    def pop(self):
        self.barrier()
        guards, tags, _ = self.scopes.pop()
        for g in reversed(guards):
            g.__exit__(None, None, None)
        for t in tags:
            del self.rot[t]

    def barrier(self):
        for e in self.STREAMS:
            waits = []
            for sid, v in self.latest.items():
                if self.waited[e].get(sid, 0) >= v:
                    continue
                self.waited[e][sid] = v
                waits.append((sid, v))
            self.ops[e].append((None, waits, None, False))

    def ps(self, name, shape=(128, 512), dtype=F32):
        return Tile(self.nc.alloc_psum_tensor(name, list(shape), dtype), name)

    def dram(self, name, shape, dtype=F32, kind="Internal"):
        return Tile(self.nc.dram_tensor(name, list(shape), dtype, kind=kind), name)

    def pool(self, tag, n, shape, dtype=F32, space="sb"):
        self.rot[tag] = [[(self.sb if space == "sb" else self.ps)(f"{tag}{i}", shape, dtype) for i in range(n)], 0]
        if self.scopes and space == "sb":
            self.scopes[-1][1].append(tag)

    def get(self, tag):
        r = self.rot[tag]
        t = r[0][r[1] % len(r[0])]
        r[1] += 1
        return t

    def _sem(self, semid):
        if semid not in self.sems:
            self.sems[semid] = self.nc.alloc_semaphore("s_" + "_".join(str(x) for x in semid))
        return self.sems[semid]

    def _states(self, key, create):
        tid, sub = key
        d = self.state.get(tid)
        if d is None:
            if not create:
                return []
            d = self.state[tid] = {}
        if sub is None:
            if create and None not in d:
                d[None] = [None, {}]
            return list(d.values())
        out = []
        if None in d:
            out.append(d[None])
        if sub in d:
            out.append(d[sub])
        elif create:
            d[sub] = [None, {}]
            out.append(d[sub])
        return out

    def op(self, eng, fn, r=(), w=(), dma=False):
        r = [_key(k) for k in r]
        w = [_key(k) for k in w]
        deps = {}

        def need(sig):
            if sig is not None and deps.get(sig[0], 0) < sig[1]:
                deps[sig[0]] = sig[1]
        for k in r:
            for st in self._states(k, False):
                need(st[0])
        for k in w:
            for st in self._states(k, False):
                need(st[0])
                for s, v in st[1].items():
                    need((s, v))
        if dma:
            n = self.cd[eng]
            self.cd[eng] += 1
            sig = (("d", eng, n % NRING), 16 * (n // NRING + 1))
            if n >= NRING:
                need((sig[0], sig[1] - 16))
        else:
            n = self.cc[eng]
            self.cc[eng] += 1
            sig = (("c", eng, n // EPOCH), n % EPOCH + 1)
        waits = []
        for s, v in deps.items():
            if eng == "pe" and s[0] == "c" and s[1] == "pe":
                continue
            if self.waited[eng].get(s, 0) >= v:
                continue
            self.waited[eng][s] = v
            waits.append((s, v))
        for k in r:
            self._states(k, True)
            tgt = self.state[k[0]][k[1]]
            if tgt[1].get(sig[0], 0) < sig[1]:
                tgt[1][sig[0]] = sig[1]
        for k in w:
            self._states(k, True)
            if k[1] is None:
                self.state[k[0]] = {None: [sig, {}]}
            else:
                self.state[k[0]][k[1]] = [sig, {}]
        self.ops[eng].append((fn, waits, sig, dma))
        self.latest[sig[0]] = sig[1]
        return sig

    def dma(self, out, in_, r=(), w=(), q="sp", **kw):
        return self.op(q, lambda e: e.dma_start(out=out, in_=in_, **kw), r, w, dma=True)

    def mm(self, out, lhsT, rhs, start=True, stop=True, r=(), w=(), **kw):
        return self.op("pe", lambda e: e.matmul(out, lhsT, rhs, start=start, stop=stop, **kw), r, w)

    def tr(self, out, in_, ident, r=(), w=()):
        return self.op("pe", lambda e: e.transpose(out, in_, ident), r, w)

    def act(self, out, in_, func, r=(), w=(), **kw):
        return self.op("act", lambda e: e.activation(out, in_, func, **kw), r, w)

    def v(self, eng, name, *args, r=(), w=(), **kw):
        return self.op(eng, lambda e: getattr(e, name)(*args, **kw), r, w)

    def emit(self, final_waits=()):
        nc = self.nc
        for e in self.ops:
            for fn, waits, sig, dma in self.ops[e]:
                if sig is not None:
                    self._sem(sig[0])
                for s, v in waits:
                    self._sem(s)
        with nc.Block() as block:
            def body(ename, extra=None):
                def f(eng):
                    for fn, waits, sig, dma in self.ops[ename]:
                        for s, v in waits:
                            eng.wait_ge(self.sems[s], v)
                        if fn is not None:
                            fn(eng).then_inc(self.sems[sig[0]], 16 if dma else 1)
                    for s, v in (extra or ()):
                        eng.wait_ge(self.sems[s], v)
                return f
            block.tensor(body("pe"))
            block.scalar(body("act"))
            block.vector(body("dve"))
            block.gpsimd(body("pool"))
            block.sync(body("sp", list(final_waits)))


class _Stop(Exception):
    pass


STOP = 0


def build(S):
    def stage(n):
        if STOP == n:
            raise _Stop()
    nc = bass.Bass("TRN2", target_bir_lowering=False)
    P = Prog(nc)
    NCH = S // 64
    T = 2 * S
    TT = min(512, T)
    NSUB = TT // 128
    NTT = T // TT
    L = DEPTH

    def din(name, shape, dt=F32):
        return P.dram(name, shape, dt, kind="ExternalInput")

    x_in = din("x_in", [T, D])
    pT = din("pT", [L, 256, T])
    prm = {}
    for name, shape in [("ln_in_w", [1, D]), ("ln_in_b", [1, D]), ("w_in", [L, D, NIN]), ("rwkv_mu", [L, 1024]),
                        ("rwkv_w0", [L, 256]), ("rwkv_w2", [L, 64, 256]), ("rwkv_a0", [L, 256]), ("rwkv_a2", [L, 64, 256]),
                        ("rwkv_g2", [L, 128, 256]), ("rwkv_k_k", [L, 256]), ("rwkv_k_a", [L, 256]), ("rwkv_r_k", [L, 256]),
                        ("rwkv_ln_w", [L, 256]), ("rwkv_ln_b", [L, 256]), ("gla_w2", [L, 16, 128]), ("gla_b", [L, 128]),
                        ("gla_norm_w", [L, 256]), ("hgrn_lb_logits", [L, 256]), ("hgrn_norm_w", [L, 256]),
                        ("w_branch", [L, 4, 256, D]), ("w_mix_out", [L, D, D]), ("ln_mix_w", [L, D]), ("ln_mix_b", [L, D]),
                        ("w_ffn_up", [L, D, 2 * DFF]), ("ffn_convT", [L, 128, 22, 3]), ("w_ffn_down", [L, DFF, D]),
                        ("w_ple_gate", [L, D, D]), ("w_ple_proj", [L, 256, D]), ("ln_ffn_w", [L, D]), ("ln_ffn_b", [L, D]),
                        ("c_cos", [T, 16]), ("c_sin", [T, 16]), ("c_maskI", [128, 512]), ("c_maskS", [128, 512]),
                        ("c_maskST", [128, 512]), ("c_bones", [128, 128]), ("c_bsel", [128, 2]), ("c_hmask", [128, 2, 512]),
                        ("c_ident", [128, 128]), ("c_retq", [128, 256]), ("c_retk", [128, 256]), ("c_retkh", [128, 256]),
                        ("c_retdec", [128, 4])]:
        prm[name] = din(name, shape)
    out_d = P.dram("out", [T, D], F32, kind="ExternalOutput")
    xres = P.dram("xres", [T, D])
    zmix = P.dram("zmix", [T, NMIX])
    oTd = P.dram("oTd", [1024, T], BF16)

    xT = None
    xTd = P.dram("xTd", [128, 8 * T], BF16)

    def xT_alloc(load):
        nonlocal xT
        xT = P.sb("xT", [128, 8, T], BF16)
        if load:
            for k in range(8):
                P.dma(xT[:, k, :], xTd.ap()[:, k * T:(k + 1) * T], r=[xTd], w=[xT])

    def xT_spill():
        for k in range(8):
            P.dma(xTd.ap()[:, k * T:(k + 1) * T], xT[:, k, :], r=[xT], w=[xTd])
    ident = P.sb("ident", [128, 128])
    maskI = P.sb("maskI", [128, 512])
    maskS = P.sb("maskS", [128, 512])
    maskST = P.sb("maskST", [128, 512])
    bones = P.sb("bones", [128, 128])
    bsel = P.sb("bsel", [128, 2])
    hmask = P.sb("hmask", [128, 2, 512])
    retq = P.sb("retq", [128, 256])
    retk = P.sb("retk", [128, 256])
    retkh = P.sb("retkh", [128, 256])
    retdec = P.sb("retdec", [128, 4])
    cosT = P.sb("cosT", [128, NCH, 16])
    sinT = P.sb("sinT", [128, NCH, 16])
    P.pool("ps", 8, [128, 512], F32, "ps")
    _pst = P.rot["ps"][0]
    for i_, tg in enumerate(["ps_rw", "ps_ret", "ps_gla", "ps_hg"]):
        P.rot[tg] = [[_pst[2 * i_], _pst[2 * i_ + 1]], 0]
    cur_ps = ["ps"]

    def PS():
        return P.get(cur_ps[0])
    lnw = lnb = arena = None

    def ln_alloc(nb=1):
        nonlocal lnw, lnb
        lnw = P.sb("lnw", [128, D])
        lnb = P.sb("lnb", [128, D])
        P.pool("xt", nb, [128, D])
        P.pool("xo", nb, [128, D])
        P.pool("xb", nb, [128, D])
        P.pool("st6", 2, [128, 2, 6])
        P.pool("mv", 2, [128, 2])
        P.pool("rs", 2, [128, 1])

    cst = [ident, maskI, maskS, maskST, bones, bsel, hmask, retq, retk, retkh, retdec]
    for t, n in zip(cst, ["c_ident", "c_maskI", "c_maskS", "c_maskST", "c_bones", "c_bsel", "c_hmask", "c_retq", "c_retk",
                          "c_retkh", "c_retdec"]):
        P.dma(t.h.ap(), prm[n].ap(), w=[t])
    P.dma(cosT[:, :, :], prm["c_cos"].ap().rearrange("(c p) i -> p c i", p=128), w=[cosT])
    P.dma(sinT[:, :, :], prm["c_sin"].ap().rearrange("(c p) i -> p c i", p=128), w=[sinT])

    def bcast_load(dst, src_row_ap, n):
        P.dma(dst, src_row_ap.partition_broadcast(128), w=[n])

    def layer_norm(xt, tok0, dst_dram, eps=1e-5):
        st6 = P.get("st6")
        mv = P.get("mv")
        rs = P.get("rs")
        for hh in range(2):
            P.v("dve", "bn_stats", st6[:, hh, :], xt[:, hh * 512:(hh + 1) * 512], r=[xt], w=[st6])
        P.v("dve", "bn_aggr", mv[:, :], st6[:, :, :].rearrange("p a b -> p (a b)"), r=[st6], w=[mv])
        P.act(rs[:, :], mv[:, 1:2], AF.Sqrt, r=[mv], w=[rs], bias=epst[:, 0:1], scale=1.0)
        P.v("dve", "reciprocal", rs[:, :], rs[:, :], r=[rs], w=[rs])
        xo = P.get("xo")
        P.v("dve", "tensor_scalar", xo[:, :], xt[:, :], mv[:, 0:1], rs[:, 0:1], ALU.subtract, ALU.mult, r=[xt, mv, rs], w=[xo])
        P.v("dve", "tensor_tensor", xo[:, :], xo[:, :], lnw[:, :], ALU.mult, r=[xo, lnw], w=[xo])
        P.v("dve", "tensor_tensor", xo[:, :], xo[:, :], lnb[:, :], ALU.add, r=[xo, lnb], w=[xo])
        sig = P.dma(dst_dram.ap()[tok0:tok0 + 128, :], xo[:, :], r=[xo], w=[dst_dram.k(tok0)])
        for g in range(2):
            ps = PS()
            for kk in range(4):
                k8 = g * 4 + kk
                P.tr(ps[:, kk * 128:(kk + 1) * 128], xo[:, k8 * 128:(k8 + 1) * 128], ident[:, :], r=[xo, ident], w=[ps])
            P.act(xT[:, g * 4:(g + 1) * 4, tok0:tok0 + 128], ps[:, :].rearrange("p (k t) -> p k t", k=4), AF.Copy,
                  r=[ps], w=[xT.k(tok0 // 128)])
        return sig

    epst = P.sb("epst", [128, 4])
    P.v("dve", "memset", epst[:, 0:1], 1e-5, w=[epst])
    P.v("dve", "memset", epst[:, 1:2], 1e-6, w=[epst])
    P.v("dve", "memset", epst[:, 2:3], 64e-5, w=[epst])
    P.v("dve", "memset", epst[:, 3:4], 1e-12, w=[epst])

    P.push()
    xT_alloc(False)
    P.push()
    ln_alloc(3)
    bcast_load(lnw[:, :], prm["ln_in_w"].ap()[0:1, :], lnw)
    bcast_load(lnb[:, :], prm["ln_in_b"].ap()[0:1, :], lnb)
    for c in range(T // 128):
        xt = P.get("xt")
        P.dma(xt[:, :], x_in.ap()[c * 128:(c + 1) * 128, :], w=[xt])
        layer_norm(xt, c * 128, xres)
    P.pop()

    zp = mub = w2s = g2s = gw2s = None
    pb = {}
    ST = {}
    STb = {}
    wk = {}

    def mixer_alloc():
        nonlocal zp, mub, w2s, g2s, gw2s
        P.pool("zt", 2, [128, NMIX])
        P.pool("zp", 2, [128, 1024])
        mub = P.sb("mub", [128, 1024])
        for n in ["w0", "a0", "kk", "ka", "rk", "lw_", "lb_", "glab", "glanw", "hlb", "hnw"]:
            pb[n] = P.sb("pb_" + n, [128, 256])
        w2s = P.sb("w2s", [128, 256], BF16)
        g2s = P.sb("g2s", [128, 256], BF16)
        gw2s = P.sb("gw2s", [16, 128], BF16)
        for m in ["ret", "rwkv", "gla", "hgrn"]:
            ST[m] = P.sb("ST_" + m, [128, 2, 2, 256])
            STb[m] = P.sb("STb_" + m, [128, 2, 2, 256], BF16)

    def W(name, shape=(128, 256), dt=F32):
        if name not in wk:
            wk[name] = P.sb("wk_" + name, list(shape), dt)
        return wk[name]


    def tt(out, a, b, op, r, w, eng="dve"):
        P.v(eng, "tensor_tensor", out, a, b, op, r=r, w=w)

    def h3(ap, h=4):
        return ap.rearrange("p (h d) -> p h d", h=h)

    def bc(ap4, n=64):
        return ap4.unsqueeze(2).to_broadcast([128, ap4.shape[1], n])

    def to_fm(src, name, dt=BF16):
        ps = PS()
        for kt in range(2):
            P.tr(ps[:, kt * 128:(kt + 1) * 128], src[:, kt * 128:(kt + 1) * 128], ident[:, :], r=[src, ident], w=[ps])
        dst = W(name, (128, 2, 128), dt)
        P.act(dst[:, :, :], ps[:, 0:256].rearrange("p (k t) -> p k t", k=2), AF.Copy, r=[ps], w=[dst])
        return dst

    def scores(lT, rT, mask, name, dt=BF16):
        pss = [PS(), PS()]
        for h in range(4):
            rows = slice(64 * (h % 2), 64 * (h % 2) + 64)
            ps = pss[h % 2]
            P.mm(ps[:, (h // 2) * 128:(h // 2 + 1) * 128], lT[rows, h // 2, :], rT[rows, h // 2, :], r=[lT, rT], w=[ps])
        dst = W(name, (128, 512), dt)
        d4 = dst[:, :].rearrange("p (g e t) -> p g e t", g=2, e=2)
        m4 = mask[:, :].rearrange("p (g e t) -> p g e t", g=2, e=2)
        for e in range(2):
            tt(d4[:, :, e, :], pss[e][:, 0:256].rearrange("p (g t) -> p g t", g=2), m4[:, :, e, :], ALU.mult,
               r=[pss[e], mask], w=[dst])
        return dst

    def cross(ps, xFM, stb, first=True):
        for b in range(2):
            for kt in range(2):
                P.mm(ps[64 * b:64 * b + 64, 0:256], xFM[:, kt, 64 * b:64 * b + 64], stb[:, kt, b, :],
                     start=(kt == 0), stop=False, r=[xFM, stb], w=[ps], skip_group_check=True)

    def state_update(m, pairs, decay):
        st, stb = ST[m], STb[m]
        for kt in range(2):
            psb = [PS(), PS()]
            tmp = W(m + "_sutmp", (128, 512))
            for b in range(2):
                ps = psb[b]
                for i, (lh, rh) in enumerate(pairs):
                    P.mm(ps[:, 0:256], lh[64 * b:64 * b + 64, kt * 128:(kt + 1) * 128], rh[64 * b:64 * b + 64, 0:256],
                         start=(i == 0), stop=(i == len(pairs) - 1), r=[lh, rh], w=[ps], skip_group_check=True)
                tt(tmp[:, b * 256:(b + 1) * 256], ps[:, 0:256], hmask[:, kt, 0:256], ALU.mult, r=[ps, hmask], w=[tmp])
            for b in range(2):
                P.v("dve", "scalar_tensor_tensor", st[:, kt, b, :], st[:, kt, b, :], decay(kt, b), tmp[:, b * 256:(b + 1) * 256],
                    ALU.mult, ALU.add, r=[st, tmp] + decay.r, w=[st])
        P.act(stb[:, :, :, :].rearrange("p a b c -> p (a b c)"), st[:, :, :, :].rearrange("p a b c -> p (a b c)"), AF.Copy,
              r=[st], w=[stb])

    class Dec:
        def __init__(self, fn, r):
            self.fn, self.r = fn, r

        def __call__(self, kt, b):
            return self.fn(kt, b)

    def rstd_of(sumsq, eps_col, name):
        t = W(name, (128, 4))
        P.act(t[:, :], sumsq[:, :], AF.Sqrt, r=[sumsq, epst], w=[t], bias=epst[:, eps_col:eps_col + 1], scale=1.0 / 64)
        P.v("dve", "reciprocal", t[:, :], t[:, :], r=[t], w=[t])
        return t

    def head_sumsq(src, name):
        sq = W(name + "_sq")
        P.act(sq[:, :], src[:, :], AF.Square, r=[src], w=[sq])
        s = W(name + "_s", (128, 4))
        P.v("dve", "tensor_reduce", s[:, :], h3(sq[:, :]), AX.X, ALU.add, r=[sq], w=[s])
        return s

    def cumsum_decay(la, name):
        ps = PS()
        P.mm(ps[:, 0:256], maskI[:, 0:128], la[:, :], r=[maskI, la], w=[ps])
        P.mm(ps[:, 256:512], bones[:, :], la[:, :], r=[bones, la], w=[ps])
        lac = W(name + "_lac", (128, 512))
        P.v("dve", "tensor_copy", lac[:, :], ps[:, :], r=[ps], w=[lac])
        ps2 = PS()
        for kt in range(2):
            P.mm(ps2[:, kt * 2:kt * 2 + 2], la[:, kt * 128:(kt + 1) * 128], bsel[:, :], r=[la, bsel], w=[ps2])
        dec = W(name + "_dec", (128, 4))
        P.act(dec[:, :], ps2[:, 0:4], AF.Exp, r=[ps2], w=[dec])
        return lac, dec

    def gla_like(m, zt, q_ap, k_ap, v_ap, la, qscale, c):
        yield
        lac, dec = cumsum_decay(la, m)
        e1 = W(m + "_e1")
        yield
        P.act(e1[:, :], lac[:, 0:256], AF.Exp, r=[lac], w=[e1])
        qt_ = W(m + "_qt")
        yield
        P.v("dve", "scalar_tensor_tensor", qt_[:, :], q_ap[0], qscale, e1[:, :], ALU.mult, ALU.mult, r=[q_ap[1], e1], w=[qt_])
        e2 = W(m + "_e2")
        yield
        P.act(e2[:, :], lac[:, 0:256], AF.Exp, r=[lac], w=[e2], scale=-1.0)
        kt_ = W(m + "_kt")
        yield
        tt(kt_[:, :], k_ap[0], e2[:, :], ALU.mult, r=[k_ap[1], e2], w=[kt_])
        d3 = W(m + "_d3")
        yield
        tt(d3[:, :], lac[:, 256:512], lac[:, 0:256], ALU.subtract, r=[lac], w=[d3])
        yield
        P.act(d3[:, :], d3[:, :], AF.Exp, r=[d3], w=[d3])
        kh = W(m + "_kh", (128, 256), BF16)
        yield
        tt(kh[:, :], k_ap[0], d3[:, :], ALU.mult, r=[k_ap[1], d3], w=[kh])
        vb = W(m + "_vb", (128, 256), BF16)
        yield
        P.act(vb[:, :], v_ap[0], AF.Copy, r=[v_ap[1]], w=[vb])
        o = yield from la_core(m, qt_, kt_, kh, vb, Dec(lambda kt, b: dec[:, kt * 2 + b:kt * 2 + b + 1], [dec]))
        return o

    def la_core(m, qt_, kt_, kh, vb, decay):
        yield
        qT = to_fm(qt_, m + "_qT")
        yield
        kT = to_fm(kt_, m + "_kT")
        yield
        A = scores(kT, qT, maskI, m + "_A")
        ps = PS()
        yield
        cross(ps, qT, STb[m])
        for h in range(4):
            yield
            P.mm(ps[:, h * 64:(h + 1) * 64], A[:, h * 128:(h + 1) * 128], vb[:, h * 64:(h + 1) * 64], start=False, stop=(h == 3),
                 r=[A, vb], w=[ps], skip_group_check=True)
        o = W(m + "_o")
        yield
        P.v("dve", "tensor_copy", o[:, :], ps[:, 0:256], r=[ps], w=[o])
        yield
        state_update(m, [(kh, vb)], decay)
        return o

    def finish_branch(n, ob, c):
        oT = to_fm(ob, "oT%d" % n)
        P.dma(oTd.ap()[n * 256:(n + 1) * 256, c * 128:(c + 1) * 128].rearrange("(k p) t -> p k t", p=128), oT[:, :, :],
              r=[oT], w=[oTd.k((n, c))])

    final = []
    try:
        stage(1)
        for l in range(L):
            P.push()
            P.pool("wab", 2, [128, 8, 512], BF16)
            P.pool("zst", 4, [128, 512])
            nblk = (NMIX + 511) // 512
            for cb in range(nblk):
                c0, c1 = cb * 512, min(NMIX, cb * 512 + 512)
                wab = P.get("wab")
                P.dma(wab[:, :, 0:c1 - c0], prm["w_in"].ap()[l, :, c0:c1].rearrange("(k p) n -> p k n", p=128), w=[wab], q="pool")
                for c in range(NCH):
                    tok0 = c * 128
                    ps = PS()
                    for k in range(8):
                        P.mm(ps[:, 0:c1 - c0], xT[:, k, tok0:tok0 + 128], wab[:, k, 0:c1 - c0], start=(k == 0), stop=(k == 7),
                             r=[xT.k(c), wab], w=[ps])
                    zst = P.get("zst")
                    if c % 2 == 0:
                        P.act(zst[:, 0:c1 - c0], ps[:, 0:c1 - c0], AF.Copy, r=[ps], w=[zst])
                    else:
                        P.v("dve", "tensor_copy", zst[:, 0:c1 - c0], ps[:, 0:c1 - c0], r=[ps], w=[zst])
                    P.dma(zmix.ap()[tok0:tok0 + 128, c0:c1], zst[:, 0:c1 - c0], r=[zst], w=[zmix.k(c)])
            P.pop()
            xT_spill()
            P.pop()

            stage(2)
            P.push()
            wk = {}
            mixer_alloc()
            bcast_load(mub[:, :], prm["rwkv_mu"].ap()[l:l + 1, :], mub)
            for n, src in [("w0", "rwkv_w0"), ("a0", "rwkv_a0"), ("kk", "rwkv_k_k"), ("ka", "rwkv_k_a"), ("rk", "rwkv_r_k"),
                           ("lw_", "rwkv_ln_w"), ("lb_", "rwkv_ln_b"), ("glanw", "gla_norm_w"),
                           ("hnw", "hgrn_norm_w")]:
                bcast_load(pb[n][:, :], prm[src].ap()[l:l + 1, :], pb[n])
            bcast_load(pb["glab"][:, 0:128], prm["gla_b"].ap()[l:l + 1, :], pb["glab"])
            if l == 0:
                P.v("dve", "memset", pb["hlb"][:, :], 0.0, w=[pb["hlb"]])
            else:
                hl0 = W("hl0")
                bcast_load(hl0[:, :], prm["hgrn_lb_logits"].ap()[0:1, :], hl0)
                bcast_load(pb["hlb"][:, :], prm["hgrn_lb_logits"].ap()[1:2, :], pb["hlb"])
                tt(pb["hlb"][:, :], pb["hlb"][:, :], hl0[:, :], ALU.subtract, r=[pb["hlb"], hl0], w=[pb["hlb"]])
                P.act(pb["hlb"][:, :], pb["hlb"][:, :], AF.Sigmoid, r=[pb["hlb"]], w=[pb["hlb"]])
            P.dma(w2s[0:64, :], prm["rwkv_w2"].ap()[l], w=[w2s], q="pool")
            P.dma(w2s[64:128, :], prm["rwkv_a2"].ap()[l], w=[w2s], q="pool")
            P.dma(g2s[:, :], prm["rwkv_g2"].ap()[l], w=[g2s], q="pool")
            P.dma(gw2s[:, :], prm["gla_w2"].ap()[l], w=[gw2s], q="pool")
            for m in ST:
                P.v("dve", "memset", ST[m][:, :, :, :].rearrange("p a b c -> p (a b c)"), 0.0, w=[ST[m]])
                P.v("dve", "memset", STb[m][:, :, :, :].rearrange("p a b c -> p (a b c)"), 0.0, w=[STb[m]])

            for c in range(NCH):
                tok0 = c * 128
                zt = P.get("zt")
                zp = P.get("zp")
                P.dma(zt[:, :], zmix.ap()[tok0:tok0 + 128, :], r=[zmix.k(c)], w=[zt])
                P.dma(zp[1:128, :], zmix.ap()[tok0:tok0 + 127, OFF_RWKV:OFF_RWKV + 1024], r=[zmix.k(c)], w=[zp])
                if c > 0:
                    P.dma(zp[0:1, :], zmix.ap()[tok0 - 65:tok0 - 64, OFF_RWKV:OFF_RWKV + 1024], r=[zmix.k(c - 1)], w=[zp])
                    P.dma(zp[64:65, :], zmix.ap()[tok0 - 1:tok0, OFF_RWKV:OFF_RWKV + 1024], r=[zmix.k(c - 1)], w=[zp])
                else:
                    P.v("dve", "memset", zp[0:1, :], 0.0, w=[zp])
                    P.v("dve", "memset", zp[64:65, :], 0.0, w=[zp])

                def ret_gen():
                    qr = W("ret_qr")
                    kr = W("ret_kr")
                    if c == 0:
                        yield
                        P.v("dve", "memset", qr[:, :], 0.0, w=[qr])
                        yield
                        P.v("dve", "memset", kr[:, :], 0.0, w=[kr])
                    cs = cosT[:, c, :].unsqueeze(1).to_broadcast([128, 4, 16])
                    sn = sinT[:, c, :].unsqueeze(1).to_broadcast([128, 4, 16])
                    for src_off, dstt in [(OFF_RET, qr), (OFF_RET + 128, kr)]:
                        src = zt[:, src_off:src_off + 128].rearrange("p (h i two) -> p h i two", h=4, two=2)
                        x1, x2 = src[:, :, :, 0], src[:, :, :, 1]
                        dv = dstt[:, :].rearrange("p (h i two) -> p h i two", h=4, two=2)
                        o1, o2 = dv[:, :, 0:16, 0], dv[:, :, 0:16, 1]
                        t1, t2 = W("rot_t1", (128, 4, 16)), W("rot_t2", (128, 4, 16))
                        yield
                        tt(t1[:, :, :], x1, cs, ALU.mult, r=[zt, cosT], w=[t1])
                        yield
                        tt(t2[:, :, :], x2, sn, ALU.mult, r=[zt, sinT], w=[t2])
                        yield
                        tt(o1, t1[:, :, :], t2[:, :, :], ALU.subtract, r=[t1, t2], w=[dstt])
                        t3, t4 = W("rot_t3", (128, 4, 16)), W("rot_t4", (128, 4, 16))
                        yield
                        tt(t3[:, :, :], x1, sn, ALU.mult, r=[zt, sinT], w=[t3])
                        yield
                        tt(t4[:, :, :], x2, cs, ALU.mult, r=[zt, cosT], w=[t4])
                        yield
                        tt(o2, t3[:, :, :], t4[:, :, :], ALU.add, r=[t3, t4], w=[dstt])
                    qt_ = W("ret_qt")
                    yield
                    tt(qt_[:, :], qr[:, :], retq[:, :], ALU.mult, r=[qr, retq], w=[qt_])
                    kt_ = W("ret_kt")
                    yield
                    tt(kt_[:, :], kr[:, :], retk[:, :], ALU.mult, r=[kr, retk], w=[kt_])
                    kh = W("ret_kh", (128, 256), BF16)
                    yield
                    tt(kh[:, :], kr[:, :], retkh[:, :], ALU.mult, r=[kr, retkh], w=[kh])
                    vb = W("ret_vb", (128, 256), BF16)
                    yield
                    P.act(vb[:, :], zt[:, OFF_RET + 256:OFF_RET + 512], AF.Copy, r=[zt], w=[vb])
                    o = yield from la_core("ret", qt_, kt_, kh, vb, Dec(lambda kt, b: retdec[:, kt:kt + 1], [retdec]))
                    sm = W("ret_sm", (128, 4))
                    yield
                    P.v("dve", "tensor_reduce", sm[:, :], h3(o[:, :]), AX.X, ALU.add, r=[o], w=[sm])
                    yield
                    P.v("dve", "tensor_scalar", sm[:, :], sm[:, :], 1.0 / 64, None, ALU.mult, r=[sm], w=[sm])
                    oc = W("ret_oc")
                    yield
                    tt(h3(oc[:, :]), h3(o[:, :]), bc(sm[:, :]), ALU.subtract, r=[o, sm], w=[oc])
                    yield
                    ss = head_sumsq(oc, "ret_ss")
                    yield
                    rs_ = rstd_of(ss, 1, "ret_rs4")
                    on = W("ret_on")
                    yield
                    tt(h3(on[:, :]), h3(oc[:, :]), bc(rs_[:, :]), ALU.mult, r=[oc, rs_], w=[on])
                    sg = W("ret_sg")
                    yield
                    P.act(sg[:, :], zt[:, OFF_RET + 512:OFF_RET + 768], AF.Silu, r=[zt], w=[sg])
                    ob = W("ret_ob")
                    yield
                    tt(ob[:, :], on[:, :], sg[:, :], ALU.mult, r=[on, sg], w=[ob])
                    yield
                    finish_branch(0, ob, c)


                def gla_gen():
                    zg = OFF_GLA
                    glT = W("gla_glT", (16, 128), BF16)
                    psg = PS()
                    yield
                    P.tr(psg[0:16, 0:128], zt[:, zg + 512:zg + 528], ident[:, :], r=[zt, ident], w=[psg])
                    yield
                    P.act(glT[:, :], psg[0:16, 0:128], AF.Copy, r=[psg], w=[glT])
                    psg2 = PS()
                    yield
                    P.mm(psg2[:, 0:128], glT[:, :], gw2s[:, :], r=[glT, gw2s], w=[psg2])
                    ya = W("gla_y", (128, 128))
                    yield
                    tt(ya[:, :], psg2[:, 0:128], pb["glab"][:, 0:128], ALU.add, r=[psg2, pb["glab"]], w=[ya])
                    yield
                    P.act(ya[:, :], ya[:, :], AF.Exp, r=[ya], w=[ya], scale=-1.0)
                    yield
                    P.act(ya[:, :], ya[:, :], AF.Ln, r=[ya], w=[ya], bias=1.0, scale=1.0)
                    la = W("gla_la")
                    qg = W("gla_q")
                    kg = W("gla_k")
                    if c == 0:
                        for t_ in (la, qg, kg):
                            yield
                            P.v("dve", "memset", t_[:, :], 0.0, w=[t_])
                    yield
                    P.v("dve", "tensor_scalar", h3(la[:, :])[:, :, 0:32], h3(ya[:, :], 4), -1.0 / 16, None, ALU.mult, r=[ya], w=[la])
                    yield
                    P.v("dve", "tensor_copy", h3(qg[:, :])[:, :, 0:32], h3(zt[:, zg:zg + 128]), r=[zt], w=[qg])
                    yield
                    P.act(h3(kg[:, :])[:, :, 0:32], h3(zt[:, zg + 128:zg + 256]), AF.Copy, r=[zt], w=[kg])
                    o = yield from gla_like("gla", zt, (qg[:, :], qg), (kg[:, :], kg), (zt[:, zg + 256:zg + 512], zt), la, 32.0 ** -0.5, c)
                    yield
                    ss = head_sumsq(o, "gla_ss")
                    yield
                    rs_ = rstd_of(ss, 1, "gla_rs4")
                    on = W("gla_on")
                    yield
                    tt(h3(on[:, :]), h3(o[:, :]), bc(rs_[:, :]), ALU.mult, r=[o, rs_], w=[on])
                    yield
                    tt(on[:, :], on[:, :], pb["glanw"][:, :], ALU.mult, r=[on, pb["glanw"]], w=[on])
                    sg = W("gla_sg")
                    yield
                    P.act(sg[:, :], zt[:, zg + 528:zg + 784], AF.Silu, r=[zt], w=[sg])
                    ob = W("gla_ob")
                    yield
                    tt(ob[:, :], on[:, :], sg[:, :], ALU.mult, r=[on, sg], w=[ob])
                    yield
                    finish_branch(2, ob, c)


                def hgrn_gen():
                    zh = OFF_HGRN
                    f = W("hg_f")
                    yield
                    P.act(f[:, :], zt[:, zh + 256:zh + 512], AF.Sigmoid, r=[zt], w=[f])
                    tmp = W("hg_tmp")
                    yield
                    tt(tmp[:, :], f[:, :], pb["hlb"][:, :], ALU.mult, r=[f, pb["hlb"]], w=[tmp])
                    yield
                    tt(f[:, :], f[:, :], tmp[:, :], ALU.subtract, r=[f, tmp], w=[f])
                    yield
                    tt(f[:, :], f[:, :], pb["hlb"][:, :], ALU.add, r=[f, pb["hlb"]], w=[f])
                    la = W("hg_la")
                    yield
                    P.act(la[:, :], f[:, :], AF.Ln, r=[f], w=[la])
                    kh_ = W("hg_k")
                    yield
                    P.v("dve", "tensor_scalar", kh_[:, :], f[:, :], -1.0, 1.0, ALU.mult, ALU.add, r=[f], w=[kh_])
                    o = yield from gla_like("hgrn", zt, (zt[:, zh:zh + 256], zt), (kh_[:, :], kh_), (zt[:, zh + 512:zh + 768], zt), la, 1.0, c)
                    yield
                    ss = head_sumsq(o, "hg_ss")
                    yield
                    rs_ = rstd_of(ss, 1, "hg_rs4")
                    on = W("hg_on")
                    yield
                    tt(h3(on[:, :]), h3(o[:, :]), bc(rs_[:, :]), ALU.mult, r=[o, rs_], w=[on])
                    yield
                    tt(on[:, :], on[:, :], pb["hnw"][:, :], ALU.mult, r=[on, pb["hnw"]], w=[on])
                    sg = W("hg_sg")
                    yield
                    P.act(sg[:, :], zt[:, zh + 768:zh + 1024], AF.Silu, r=[zt], w=[sg])
                    ob = W("hg_ob")
                    yield
                    tt(ob[:, :], on[:, :], sg[:, :], ALU.mult, r=[on, sg], w=[ob])
                    yield
                    finish_branch(3, ob, c)


                def rwkv_gen():
                    zr = OFF_RWKV
                    zs = zp
                    yield
                    tt(zs[:, :], zp[:, :], zt[:, zr:zr + 1024], ALU.subtract, r=[zp, zt], w=[zs])
                    yield
                    tt(zs[:, :], zs[:, :], mub[:, :], ALU.mult, r=[zs, mub], w=[zs])
                    yield
                    tt(zs[:, :], zs[:, :], zt[:, zr:zr + 1024], ALU.add, r=[zs, zt], w=[zs])
                    r_, k_, v_ = zs[:, 0:256], zs[:, 256:512], zs[:, 512:768]
                    li = W("rw_li")
                    yield
                    P.act(li[:, 0:64], zs[:, 768:832], AF.Tanh, r=[zs], w=[li])
                    yield
                    P.act(li[:, 64:128], zs[:, 832:896], AF.Copy, r=[zs], w=[li])
                    yield
                    P.act(li[:, 128:256], zs[:, 896:1024], AF.Sigmoid, r=[zs], w=[li])
                    yield
                    liT = to_fm(li, "rw_liT")
                    psl = PS()
                    yield
                    P.mm(psl[:, 0:256], liT[0:64, 0, :], w2s[0:64, :], r=[liT, w2s], w=[psl])
                    lw = W("rw_lw")
                    yield
                    tt(lw[:, :], psl[:, 0:256], pb["w0"][:, :], ALU.add, r=[psl, pb["w0"]], w=[lw])
                    pslb = PS()
                    yield
                    P.mm(pslb[:, 0:256], liT[64:128, 0, :], w2s[64:128, :], r=[liT, w2s], w=[pslb])
                    a_ = W("rw_a")
                    yield
                    tt(a_[:, :], pslb[:, 0:256], pb["a0"][:, :], ALU.add, r=[pslb, pb["a0"]], w=[a_])
                    psl2 = PS()
                    yield
                    P.mm(psl2[:, 0:256], liT[:, 1, :], g2s[:, :], r=[liT, g2s], w=[psl2])
                    g_ = W("rw_g")
                    yield
                    P.act(g_[:, :], psl2[:, 0:256], AF.Copy, r=[psl2], w=[g_])
                    yield
                    P.act(lw[:, :], lw[:, :], AF.Sigmoid, r=[lw], w=[lw])
                    yield
                    P.v("dve", "tensor_scalar", lw[:, :], lw[:, :], -0.6065306597126334, None, ALU.mult, r=[lw], w=[lw])
                    yield
                    P.act(a_[:, :], a_[:, :], AF.Sigmoid, r=[a_], w=[a_])
                    kk = W("rw_kk")
                    yield
                    tt(kk[:, :], k_, pb["kk"][:, :], ALU.mult, r=[zs, pb["kk"]], w=[kk])
                    sq = W("rw_t1")
                    yield
                    P.act(sq[:, :], kk[:, :], AF.Square, r=[kk], w=[sq])
                    s4 = W("rw_s4", (128, 4))
                    yield
                    P.v("dve", "tensor_reduce", s4[:, :], h3(sq[:, :]), AX.X, ALU.add, r=[sq], w=[s4])
                    rn = W("rw_rn", (128, 4))
                    yield
                    P.act(rn[:, :], s4[:, :], AF.Sqrt, r=[s4, epst], w=[rn], bias=epst[:, 3:4], scale=1.0)
                    yield
                    P.v("dve", "reciprocal", rn[:, :], rn[:, :], r=[rn], w=[rn])
                    kkn = W("rw_kkn")
                    yield
                    tt(h3(kkn[:, :]), h3(kk[:, :]), bc(rn[:, :]), ALU.mult, r=[kk, rn], w=[kkn])
                    kp = W("rw_kp")
                    yield
                    P.v("dve", "scalar_tensor_tensor", kp[:, :], a_[:, :], -1.0, pb["ka"][:, :], ALU.add, ALU.mult, r=[a_, pb["ka"]], w=[kp])
                    yield
                    P.v("dve", "scalar_tensor_tensor", kp[:, :], kp[:, :], 1.0, k_, ALU.add, ALU.mult, r=[kp, zs], w=[kp])
                    bt = W("rw_t1")
                    yield
                    tt(bt[:, :], r_, kp[:, :], ALU.mult, r=[zs, kp], w=[bt])
                    yield
                    tt(bt[:, :], bt[:, :], pb["rk"][:, :], ALU.mult, r=[bt, pb["rk"]], w=[bt])
                    b4 = W("rw_b4", (128, 4))
                    yield
                    P.v("dve", "tensor_reduce", b4[:, :], h3(bt[:, :]), AX.X, ALU.add, r=[bt], w=[b4])
                    bon = W("rw_bon")
                    yield
                    tt(h3(bon[:, :]), h3(v_), bc(b4[:, :]), ALU.mult, r=[zs, b4], w=[bon])
                    yield
                    lac, dec = cumsum_decay(lw, "rw")
                    ea = W("rw_e1")
                    yield
                    tt(ea[:, :], lac[:, 0:256], lw[:, :], ALU.subtract, r=[lac, lw], w=[ea])
                    yield
                    P.act(ea[:, :], ea[:, :], AF.Exp, r=[ea], w=[ea])
                    at = W("rw_tl")
                    yield
                    P.v("dve", "scalar_tensor_tensor", at[:, :], kkn[:, :], -1.0, ea[:, :], ALU.mult, ALU.mult, r=[kkn, ea], w=[at])
                    yield
                    aT = to_fm(at, "rw_aT")
                    bp = W("rw_bp")
                    yield
                    tt(bp[:, :], kkn[:, :], a_[:, :], ALU.mult, r=[kkn, a_], w=[bp])
                    eb = W("rw_e2")
                    yield
                    P.act(eb[:, :], lac[:, 0:256], AF.Exp, r=[lac], w=[eb], scale=-1.0)
                    btl = W("rw_tl")
                    yield
                    tt(btl[:, :], bp[:, :], eb[:, :], ALU.mult, r=[bp, eb], w=[btl])
                    yield
                    bT = to_fm(btl, "rw_bT")
                    ktl = W("rw_tl")
                    yield
                    tt(ktl[:, :], kp[:, :], eb[:, :], ALU.mult, r=[kp, eb], w=[ktl])
                    yield
                    kT = to_fm(ktl, "rw_kT")
                    er = W("rw_e1")
                    yield
                    P.act(er[:, :], lac[:, 0:256], AF.Exp, r=[lac], w=[er])
                    rtl = W("rw_tl")
                    yield
                    tt(rtl[:, :], r_, er[:, :], ALU.mult, r=[zs, er], w=[rtl])
                    yield
                    rT = to_fm(rtl, "rw_rT")
                    eh = W("rw_e2")
                    yield
                    tt(eh[:, :], lac[:, 256:512], lac[:, 0:256], ALU.subtract, r=[lac], w=[eh])
                    yield
                    P.act(eh[:, :], eh[:, :], AF.Exp, r=[eh], w=[eh])
                    bh = W("rw_bh", (128, 256), BF16)
                    yield
                    tt(bh[:, :], bp[:, :], eh[:, :], ALU.mult, r=[bp, eh], w=[bh])
                    khh = W("rw_khh", (128, 256), BF16)
                    yield
                    tt(khh[:, :], kp[:, :], eh[:, :], ALU.mult, r=[kp, eh], w=[khh])
                    vb = W("rw_vb", (128, 256), BF16)
                    yield
                    P.act(vb[:, :], v_, AF.Copy, r=[zs], w=[vb])
                    yield
                    Lm = scores(aT, bT, maskST, "rw_P", F32)
                    yield
                    LmT = scores(bT, aT, maskS, "rw_PT", F32)
                    yield
                    LakT = scores(kT, aT, maskS, "rw_LakT")
                    yield
                    MrbT = scores(bT, rT, maskI, "rw_MrbT")
                    yield
                    MrkT = scores(kT, rT, maskI, "rw_MrkT")
                    ps = PS()
                    yield
                    cross(ps, aT, STb["rwkv"])
                    for h in range(4):
                        yield
                        P.mm(ps[:, h * 64:(h + 1) * 64], LakT[:, h * 128:(h + 1) * 128], vb[:, h * 64:(h + 1) * 64], start=False, stop=(h == 3),
                             r=[LakT, vb], w=[ps], skip_group_check=True)
                    X = W("rw_X0")
                    yield
                    P.v("dve", "tensor_copy", X[:, :], ps[:, 0:256], r=[ps], w=[X])
                    Pm, PmT = Lm, LmT
                    for lev in range(6):
                        psx = PS()
                        for h in range(4):
                            yield
                            P.mm(psx[:, h * 64:(h + 1) * 64], PmT[:, h * 128:(h + 1) * 128], X[:, h * 64:(h + 1) * 64], r=[PmT, X], w=[psx])
                        Xn = W("rw_X0")
                        yield
                        tt(Xn[:, :], X[:, :], psx[:, 0:256], ALU.add, r=[X, psx], w=[Xn])
                        X = Xn
                        if lev < 5:
                            psp = PS()
                            pspt = PS()
                            for h in range(4):
                                hs = slice(h * 128, (h + 1) * 128)
                                yield
                                P.mm(psp[:, hs], PmT[:, hs], Pm[:, hs], r=[PmT, Pm], w=[psp])
                                yield
                                P.mm(pspt[:, hs], Pm[:, hs], PmT[:, hs], r=[PmT, Pm], w=[pspt])
                            Pn = W("rw_P", (128, 512))
                            PnT = W("rw_PT", (128, 512))
                            yield
                            P.act(Pn[:, :], psp[:, :], AF.Copy, r=[psp], w=[Pn])
                            yield
                            P.v("dve", "tensor_copy", PnT[:, :], pspt[:, :], r=[pspt], w=[PnT])
                            Pm, PmT = Pn, PnT
                    ub = W("rw_ub", (128, 256), BF16)
                    yield
                    P.act(ub[:, :], X[:, :], AF.Copy, r=[X], w=[ub])
                    ps = PS()
                    yield
                    cross(ps, rT, STb["rwkv"])
                    for h in range(4):
                        yield
                        P.mm(ps[:, h * 64:(h + 1) * 64], MrbT[:, h * 128:(h + 1) * 128], ub[:, h * 64:(h + 1) * 64], start=False, stop=False,
                             r=[MrbT, ub], w=[ps], skip_group_check=True)
                        yield
                        P.mm(ps[:, h * 64:(h + 1) * 64], MrkT[:, h * 128:(h + 1) * 128], vb[:, h * 64:(h + 1) * 64], start=False, stop=(h == 3),
                             r=[MrkT, vb], w=[ps], skip_group_check=True)
                    y = W("rw_y")
                    yield
                    P.v("dve", "tensor_copy", y[:, :], ps[:, 0:256], r=[ps], w=[y])
                    yield
                    state_update("rwkv", [(bh, ub), (khh, vb)], Dec(lambda kt, b: dec[:, kt * 2 + b:kt * 2 + b + 1], [dec]))
                    sm = W("rw_sm", (128, 4))
                    yield
                    P.v("dve", "tensor_reduce", sm[:, :], h3(y[:, :]), AX.X, ALU.add, r=[y], w=[sm])
                    yield
                    P.v("dve", "tensor_scalar", sm[:, :], sm[:, :], 1.0 / 64, None, ALU.mult, r=[sm], w=[sm])
                    yc = W("rw_oc")
                    yield
                    tt(h3(yc[:, :]), h3(y[:, :]), bc(sm[:, :]), ALU.subtract, r=[y, sm], w=[yc])
                    yield
                    ss = head_sumsq(yc, "rw_ss")
                    yield
                    rs_ = rstd_of(ss, 2, "rw_rs4")
                    yn = W("rw_on")
                    yield
                    tt(h3(yn[:, :]), h3(yc[:, :]), bc(rs_[:, :]), ALU.mult, r=[yc, rs_], w=[yn])
                    yield
                    tt(yn[:, :], yn[:, :], pb["lw_"][:, :], ALU.mult, r=[yn, pb["lw_"]], w=[yn])
                    yield
                    tt(yn[:, :], yn[:, :], pb["lb_"][:, :], ALU.add, r=[yn, pb["lb_"]], w=[yn])
                    yield
                    tt(yn[:, :], yn[:, :], bon[:, :], ALU.add, r=[yn, bon], w=[yn])
                    ob = W("rw_ob")
                    yield
                    tt(ob[:, :], yn[:, :], g_[:, :], ALU.mult, r=[yn, g_], w=[ob])
                    yield
                    finish_branch(1, ob, c)

                rw = rwkv_gen()
                gens = [(rw, "ps_rw"), (ret_gen(), "ps_ret"), (gla_gen(), "ps_gla"), (hgrn_gen(), "ps_hg")]
                for g, tg in [gens[1], gens[2], gens[3], gens[0]]:
                    cur_ps[0] = "ps"
                    for _ in g:
                        pass
                cur_ps[0] = "ps"

            stage(6)
            P.pop()
            P.push()
            xT_alloc(True)
            P.push()
            ln_alloc(2)
            bcast_load(lnw[:, :], prm["ln_mix_w"].ap()[l:l + 1, :], lnw)
            bcast_load(lnb[:, :], prm["ln_mix_b"].ap()[l:l + 1, :], lnb)
            arena = P.sb("wo", [128, 8, D], BF16)
            wo = arena
            P.dma(wo[:, :, :], prm["w_mix_out"].ap()[l].rearrange("(k p) n -> p k n", p=128), w=[arena], q="pool")
            if True:
                P.pool("oTt", 2, [128, 8, TT], BF16)
                P.pool("wg", 6, [128, 8, 128], BF16)
                P.pool("wbk", 6, [128, 2, 128], BF16)
                P.pool("sgc", 2, [128, TT])
                P.pool("acc", 2, [128, TT])
                P.pool("tmpc", 2, [128, TT])
                P.pool("mT", 2, [128, 8, TT], BF16)
            for ti in range(NTT):
                t0 = ti * TT
                oTt = P.get("oTt")
                P.dma(oTt[:, :, :], oTd.ap()[:, t0:t0 + TT].rearrange("(k p) t -> p k t", p=128),
                      r=[oTd.k((n, t0 // 128 + s)) for n in range(4) for s in range(NSUB)], w=[oTt])
                mT = P.get("mT")
                for j in range(8):
                    acc = P.get("acc")
                    for n in range(4):
                        wg = P.get("wg")
                        gc0 = OFF_GATE + n * D + j * 128
                        P.dma(wg[:, :, :], prm["w_in"].ap()[l, :, gc0:gc0 + 128].rearrange("(k p) n -> p k n", p=128), w=[wg], q="pool")
                        wbk = P.get("wbk")
                        P.dma(wbk[:, :, :], prm["w_branch"].ap()[l, n, :, j * 128:(j + 1) * 128].rearrange("(k p) n -> p k n", p=128),
                              w=[wbk], q="pool")
                        psg_ = PS()
                        for k in range(8):
                            P.mm(psg_[:, 0:TT], wg[:, k, :], xT[:, k, t0:t0 + TT], start=(k == 0), stop=(k == 7),
                                 r=[wg] + [xT.k(t0 // 128 + s) for s in range(NSUB)], w=[psg_])
                        psp_ = PS()
                        for k in range(2):
                            P.mm(psp_[:, 0:TT], wbk[:, k, :], oTt[:, n * 2 + k, :], start=(k == 0), stop=(k == 1), r=[wbk, oTt], w=[psp_])
                        sgc = P.get("sgc")
                        P.act(sgc[:, :], psg_[:, 0:TT], AF.Sigmoid, r=[psg_], w=[sgc])
                        if n == 0:
                            tt(acc[:, :], sgc[:, :], psp_[:, 0:TT], ALU.mult, r=[sgc, psp_], w=[acc])
                        else:
                            tm_ = P.get("tmpc")
                            tt(tm_[:, :], sgc[:, :], psp_[:, 0:TT], ALU.mult, r=[sgc, psp_], w=[tm_])
                            tt(acc[:, :], acc[:, :], tm_[:, :], ALU.add, r=[acc, tm_], w=[acc])
                    P.act(mT[:, j, :], acc[:, :], AF.Copy, r=[acc], w=[mT])
                for s in range(NSUB):
                    tok0 = t0 + s * 128
                    xb = P.get("xb")
                    P.dma(xb[:, :], xres.ap()[tok0:tok0 + 128, :], r=[xres.k(tok0)], w=[xb])
                    xt = P.get("xt")
                    for hh in range(2):
                        ps = PS()
                        for k in range(8):
                            P.mm(ps[:, :], mT[:, k, s * 128:(s + 1) * 128], wo[:, k, hh * 512:(hh + 1) * 512], start=(k == 0), stop=(k == 7),
                                 r=[mT, arena], w=[ps])
                        P.v("dve", "scalar_tensor_tensor", xt[:, hh * 512:(hh + 1) * 512], xb[:, hh * 512:(hh + 1) * 512], ALPHA, ps[:, :],
                            ALU.mult, ALU.add, r=[xb, ps], w=[xt])
                    layer_norm(xt, tok0, xres)

            stage(7)
            P.pop()
            P.push()
            wk = {}
            ln_alloc()
            bcast_load(lnw[:, :], prm["ln_ffn_w"].ap()[l:l + 1, :], lnw)
            bcast_load(lnb[:, :], prm["ln_ffn_b"].ap()[l:l + 1, :], lnb)
            arena = P.sb("wd", [128, 22, D], BF16)
            wd = arena
            P.dma(wd[:, :, :], prm["w_ffn_down"].ap()[l].rearrange("(k p) n -> p k n", p=128), w=[arena], q="pool")
            if True:
                wk["wpg"] = P.sb("wpg", [128, 8, D], BF16)
                wk["wpp"] = P.sb("wpp", [128, 2, D], BF16)
                wk["cw"] = P.sb("cw", [128, 22, 3])
                wk["hist"] = P.sb("hist", [128, 22, 2, 2])
                P.pool("wu", 2, [128, 8, 128], BF16)
                P.pool("wv", 2, [128, 8, 128], BF16)
                P.pool("ub", 1, [128, 2, 2 + TT // 2])
                P.pool("cv", 1, [128, 2, TT // 2])
                P.pool("cv2", 1, [128, 2, TT // 2])
                P.pool("hT", 1, [128, 22, TT], BF16)
                P.pool("pTt", 1, [128, 2, TT], BF16)
                P.pool("plg", 1, [128, 512])
            wpg, wpp, cw, hist = wk["wpg"], wk["wpp"], wk["cw"], wk["hist"]
            P.dma(wpg[:, :, :], prm["w_ple_gate"].ap()[l].rearrange("(k p) n -> p k n", p=128), w=[wpg], q="pool")
            P.dma(wpp[:, :, :], prm["w_ple_proj"].ap()[l].rearrange("(k p) n -> p k n", p=128), w=[wpp], q="pool")
            P.dma(cw[:, :, :], prm["ffn_convT"].ap()[l], w=[cw])
            P.v("dve", "memset", hist[:, :, :, :].rearrange("p a b c -> p (a b c)"), 0.0, w=[hist])
            NJ = TT // 2
            for ti in range(NTT):
                t0 = ti * TT
                xk = [xT.k(t0 // 128 + s) for s in range(NSUB)]
                hT = P.get("hT")
                pTt = P.get("pTt")
                P.dma(pTt[:, :, :], pT.ap()[l, :, t0:t0 + TT].rearrange("(k p) t -> p k t", p=128), w=[pTt], q="pool")
                for i in range(22):
                    wu = P.get("wu")
                    wv = P.get("wv")
                    P.dma(wu[:, :, :], prm["w_ffn_up"].ap()[l, :, i * 128:(i + 1) * 128].rearrange("(k p) n -> p k n", p=128), w=[wu], q="pool")
                    P.dma(wv[:, :, :], prm["w_ffn_up"].ap()[l, :, DFF + i * 128:DFF + (i + 1) * 128].rearrange("(k p) n -> p k n", p=128),
                          w=[wv], q="pool")
                    psu = PS()
                    psv = PS()
                    for k in range(8):
                        P.mm(psu[:, 0:TT], wu[:, k, :], xT[:, k, t0:t0 + TT], start=(k == 0), stop=(k == 7), r=[wu] + xk, w=[psu])
                    for k in range(8):
                        P.mm(psv[:, 0:TT], wv[:, k, :], xT[:, k, t0:t0 + TT], start=(k == 0), stop=(k == 7), r=[wv] + xk, w=[psv])
                    ub = P.get("ub")
                    P.act(ub[:, :, 2:2 + NJ].rearrange("p b (c j) -> p b c j", j=64),
                          psu[:, 0:TT].rearrange("p (c b j) -> p b c j", b=2, j=64), AF.Copy, r=[psu], w=[ub])
                    P.v("dve", "tensor_copy", ub[:, :, 0:2], hist[:, i, :, :], r=[hist], w=[ub])
                    P.v("dve", "tensor_copy", hist[:, i, :, :], ub[:, :, NJ:NJ + 2], r=[ub], w=[hist])
                    cv = P.get("cv")
                    P.v("dve", "tensor_scalar", cv[:, :, :], ub[:, :, 0:NJ], cw[:, i, 0:1], None, ALU.mult, r=[ub, cw], w=[cv])
                    cv2 = P.get("cv2")
                    P.v("dve", "scalar_tensor_tensor", cv2[:, :, :], ub[:, :, 1:1 + NJ], cw[:, i, 1:2], cv[:, :, :], ALU.mult, ALU.add,
                        r=[ub, cw, cv], w=[cv2])
                    P.v("dve", "scalar_tensor_tensor", cv[:, :, :], ub[:, :, 2:2 + NJ], cw[:, i, 2:3], cv2[:, :, :], ALU.mult, ALU.add,
                        r=[ub, cw, cv2], w=[cv])
                    P.act(cv2[:, :, :], cv[:, :, :], AF.Gelu_apprx_tanh, r=[cv], w=[cv2])
                    tt(hT[:, i, :].rearrange("p (c b j) -> p b c j", b=2, j=64), cv2[:, :, :].rearrange("p b (c j) -> p b c j", j=64),
                       psv[:, 0:TT].rearrange("p (c b j) -> p b c j", b=2, j=64), ALU.mult, r=[cv2, psv], w=[hT])
                for s in range(NSUB):
                    tok0 = t0 + s * 128
                    xb = P.get("xb")
                    lastsig = None
                    P.dma(xb[:, :], xres.ap()[tok0:tok0 + 128, :], r=[xres.k(tok0)], w=[xb])
                    xt = P.get("xt")
                    for hh in range(2):
                        cs_ = slice(hh * 512, (hh + 1) * 512)
                        psg_ = PS()
                        for k in range(8):
                            P.mm(psg_[:, :], xT[:, k, tok0:tok0 + 128], wpg[:, k, cs_], start=(k == 0), stop=(k == 7),
                                 r=[xT.k(tok0 // 128), wpg], w=[psg_])
                        psq = PS()
                        for k in range(2):
                            P.mm(psq[:, :], pTt[:, k, s * 128:(s + 1) * 128], wpp[:, k, cs_], start=(k == 0), stop=(k == 1), r=[pTt, wpp], w=[psq])
                        plg = P.get("plg")
                        P.act(plg[:, :], psg_[:, :], AF.Sigmoid, r=[psg_], w=[plg])
                        tt(plg[:, :], plg[:, :], psq[:, :], ALU.mult, r=[plg, psq], w=[plg])
                        psd = PS()
                        for k in range(22):
                            P.mm(psd[:, :], hT[:, k, s * 128:(s + 1) * 128], wd[:, k, cs_], start=(k == 0), stop=(k == 21), r=[hT, arena], w=[psd])
                        tt(plg[:, :], plg[:, :], psd[:, :], ALU.add, r=[plg, psd], w=[plg])
                        P.v("dve", "scalar_tensor_tensor", xt[:, cs_], xb[:, cs_], ALPHA, plg[:, :], ALU.mult, ALU.add, r=[xb, plg], w=[xt])
                    sig = layer_norm(xt, tok0, out_d if l == L - 1 else xres)
                    if l == L - 1:
                        final.append(sig)
            P.pop()
            if l == L - 1:
                P.pop()
    except _Stop:
        final = [P.dma(out_d.ap()[0:128, 0:128], ident[:, :], r=[ident], w=[out_d.k('dbg')])]
    P.emit(final_waits=final)
    return nc


def _consts(S):
    T = 2 * S
    c = {}
    theta = (1.0 / (10000.0 ** np.linspace(0.0, 1.0, 16))).astype(np.float32)
    pos = np.arange(S, dtype=np.float32)
    ang = (pos[:, None] * theta[None, :]).astype(np.float32)
    cos, sin = np.cos(ang).astype(np.float32), np.sin(ang).astype(np.float32)
    idx = np.arange(S).reshape(S // 64, 1, 64).repeat(2, axis=1).reshape(-1)
    c["c_cos"], c["c_sin"] = cos[idx], sin[idx]
    p = np.arange(128)
    b, j = p // 64, p % 64
    same = b[:, None] == b[None, :]
    mI = (same & (j[:, None] <= j[None, :])).astype(np.float32)
    mS = (same & (j[:, None] < j[None, :])).astype(np.float32)
    c["c_maskI"] = np.tile(mI, (1, 4))
    c["c_maskS"] = np.tile(mS, (1, 4))
    c["c_maskST"] = np.tile(mS.T.copy(), (1, 4))
    c["c_bones"] = same.astype(np.float32)
    c["c_bsel"] = np.stack([(b == 0), (b == 1)], 1).astype(np.float32)
    hm = np.zeros((128, 2, 2, 4, 64), np.float32)
    for kt in range(2):
        for pp in range(128):
            hm[pp, kt, :, 2 * kt + pp // 64, :] = 1.0
    c["c_hmask"] = hm.reshape(128, 2, 512)
    c["c_ident"] = np.eye(128, dtype=np.float32)
    lg = np.log1p(-np.exp2(-5.0 - np.arange(4, dtype=np.float32))).astype(np.float32)
    jj = j.astype(np.float32)
    q = np.exp(lg[None, :] * (jj[:, None] + 1.0))
    k = np.exp(-lg[None, :] * (jj[:, None] + 1.0)) * 32.0 ** -0.5
    kh = np.exp(lg[None, :] * (63.0 - jj[:, None])) * 32.0 ** -0.5
    c["c_retq"] = np.repeat(q, 64, 1).astype(np.float32)
    c["c_retk"] = np.repeat(k, 64, 1).astype(np.float32)
    c["c_retkh"] = np.repeat(kh, 64, 1).astype(np.float32)
    dec = np.exp(lg * 64.0)
    rd = np.zeros((128, 4), np.float32)
    for kt in range(2):
        rd[:, kt] = dec[2 * kt + p // 64]
    c["c_retdec"] = rd
    return {k_: np.ascontiguousarray(v, dtype=np.float32) for k_, v in c.items()}


def prep_inputs(inputs, S, ncores):
    f = lambda a: np.ascontiguousarray(np.asarray(a, dtype=np.float32))
    x, p = f(inputs["x"]), f(inputs["p"])
    L = DEPTH
    shared = {}
    for n in ["w_in", "rwkv_mu", "rwkv_w0", "rwkv_w2", "rwkv_a0", "rwkv_a2", "rwkv_g2", "rwkv_k_k", "rwkv_k_a", "rwkv_ln_w",
              "rwkv_ln_b", "gla_w2", "gla_b", "w_branch", "w_mix_out", "ln_mix_w", "ln_mix_b", "w_ffn_up", "w_ffn_down",
              "w_ple_gate", "w_ple_proj", "ln_ffn_w", "ln_ffn_b"]:
        shared[n] = f(inputs[n])
    shared["ln_in_w"] = f(inputs["ln_in_w"]).reshape(1, D)
    shared["ln_in_b"] = f(inputs["ln_in_b"]).reshape(1, D)
    shared["rwkv_r_k"] = f(inputs["rwkv_r_k"]).reshape(L, 256)
    shared["gla_norm_w"] = np.ascontiguousarray(np.tile(f(inputs["gla_norm_w"]), (1, 4)))
    shared["hgrn_norm_w"] = np.ascontiguousarray(np.tile(f(inputs["hgrn_norm_w"]), (1, 4)))
    shared["hgrn_lb_logits"] = f(inputs["hgrn_lb_logits"])
    cv = f(inputs["ffn_conv"])
    shared["ffn_convT"] = np.ascontiguousarray(cv.reshape(L, 3, 22, 128).transpose(0, 3, 2, 1))
    shared.update(_consts(S))
    maps = []
    for c in range(ncores):
        xs = x[2 * c:2 * c + 2]
        xi = np.ascontiguousarray(xs.reshape(2, S // 64, 64, D).transpose(1, 0, 2, 3).reshape(2 * S, D))
        ps_ = p[:, 2 * c:2 * c + 2]
        pi = ps_.reshape(L, 2, S // 64, 64, 256).transpose(0, 4, 2, 1, 3).reshape(L, 256, 2 * S)
        m = dict(shared)
        m["x_in"] = xi
        m["pT"] = np.ascontiguousarray(pi)
        maps.append(m)
    return maps


def unshuffle(outs, S):
    res = []
    for o in outs:
        res.append(o.reshape(S // 64, 2, 64, D).transpose(1, 0, 2, 3).reshape(2, S, D))
    return np.concatenate(res, 0)


def kernel(**inputs):
    S = 2048
    ncores = 8
    nc = build(S)
    maps = prep_inputs(inputs, S, ncores)
    res = run_bass_kernel_spmd(nc, maps, core_ids=list(range(ncores)))
    return unshuffle([np.asarray(r["out"]) for r in res.results], S).astype(np.float32)
```

```python
import numpy as np
import concourse.bass as bass
import concourse.mybir as mybir
from concourse.bass_utils import run_bass_kernel_spmd

F32 = mybir.dt.float32
BF16 = mybir.dt.bfloat16
AF = mybir.ActivationFunctionType
ALU = mybir.AluOpType
AX = mybir.AxisListType

EPOCH = 30000
NRING = 8

D = 1024
DEPTH = 2
NIN = 7696
DFF = 2816
ALPHA = (2.0 * DEPTH) ** 0.25
OFF_RET, OFF_RWKV, OFF_GLA, OFF_HGRN, OFF_GATE = 0, 768, 1792, 2576, 3600
NMIX = 3600


class Tile:
    _n = 0

    def __init__(self, handle, name):
        self.h = handle
        self.name = name
        Tile._n += 1
        self.id = Tile._n

    def __getitem__(self, k):
        return self.h[k]

    def ap(self):
        return self.h.ap()

    def k(self, sub):
        return (self.id, sub)


def _key(k):
    return (k.id, None) if isinstance(k, Tile) else k


class Prog:
    STREAMS = ("pe", "act", "dve", "pool", "sp")

    def __init__(self, nc):
        self.nc = nc
        self.ops = {e: [] for e in self.STREAMS}
        self.cc = {e: 0 for e in self.STREAMS}
        self.cd = {e: 0 for e in self.STREAMS}
        self.state = {}
        self.waited = {e: {} for e in self.STREAMS}
        self.sems = {}
        self.rot = {}
        self.latest = {}
        self.scopes = []
        self.uid = 0

    def sb(self, name, shape, dtype=F32):
        self.uid += 1
        g = self.nc.sbuf_tensor(f"{name}_{self.uid}", list(shape), dtype)
        h = g.__enter__()
        if self.scopes:
            self.scopes[-1][0].append(g)
        return Tile(h, name)

    def push(self):
        self.scopes.append([[], [], {}])

    def pop(self):
        self.barrier()
        guards, tags, _ = self.scopes.pop()
        for g in reversed(guards):
            g.__exit__(None, None, None)
        for t in tags:
            del self.rot[t]

    def barrier(self):
        for e in self.STREAMS:
            waits = []
            for sid, v in self.latest.items():
                if self.waited[e].get(sid, 0) >= v:
                    continue
                self.waited[e][sid] = v
                waits.append((sid, v))
            self.ops[e].append((None, waits, None, False))

    def ps(self, name, shape=(128, 512), dtype=F32):
        return Tile(self.nc.alloc_psum_tensor(name, list(shape), dtype), name)

    def dram(self, name, shape, dtype=F32, kind="Internal"):
        return Tile(self.nc.dram_tensor(name, list(shape), dtype, kind=kind), name)

    def pool(self, tag, n, shape, dtype=F32, space="sb"):
        self.rot[tag] = [[(self.sb if space == "sb" else self.ps)(f"{tag}{i}", shape, dtype) for i in range(n)], 0]
        if self.scopes and space == "sb":
            self.scopes[-1][1].append(tag)

    def get(self, tag):
        r = self.rot[tag]
        t = r[0][r[1] % len(r[0])]
        r[1] += 1
        return t

    def _sem(self, semid):
        if semid not in self.sems:
            self.sems[semid] = self.nc.alloc_semaphore("s_" + "_".join(str(x) for x in semid))
        return self.sems[semid]

    def _states(self, key, create):
        tid, sub = key
        d = self.state.get(tid)
        if d is None:
            if not create:
                return []
            d = self.state[tid] = {}
        if sub is None:
            if create and None not in d:
                d[None] = [None, {}]
            return list(d.values())
        out = []
        if None in d:
            out.append(d[None])
        if sub in d:
            out.append(d[sub])
        elif create:
            d[sub] = [None, {}]
            out.append(d[sub])
        return out

    def op(self, eng, fn, r=(), w=(), dma=False):
        r = [_key(k) for k in r]
        w = [_key(k) for k in w]
        deps = {}

        def need(sig):
            if sig is not None and deps.get(sig[0], 0) < sig[1]:
                deps[sig[0]] = sig[1]
        for k in r:
            for st in self._states(k, False):
                need(st[0])
        for k in w:
            for st in self._states(k, False):
                need(st[0])
                for s, v in st[1].items():
                    need((s, v))
        if dma:
            n = self.cd[eng]
            self.cd[eng] += 1
            sig = (("d", eng, n % NRING), 16 * (n // NRING + 1))
            if n >= NRING:
                need((sig[0], sig[1] - 16))
        else:
            n = self.cc[eng]
            self.cc[eng] += 1
            sig = (("c", eng, n // EPOCH), n % EPOCH + 1)
        waits = []
        for s, v in deps.items():
            if eng == "pe" and s[0] == "c" and s[1] == "pe":
                continue
            if self.waited[eng].get(s, 0) >= v:
                continue
            self.waited[eng][s] = v
            waits.append((s, v))
        for k in r:
            self._states(k, True)
            tgt = self.state[k[0]][k[1]]
            if tgt[1].get(sig[0], 0) < sig[1]:
                tgt[1][sig[0]] = sig[1]
        for k in w:
            self._states(k, True)
            if k[1] is None:
                self.state[k[0]] = {None: [sig, {}]}
            else:
                self.state[k[0]][k[1]] = [sig, {}]
        self.ops[eng].append((fn, waits, sig, dma))
        self.latest[sig[0]] = sig[1]
        return sig

    def dma(self, out, in_, r=(), w=(), q="sp", **kw):
        return self.op(q, lambda e: e.dma_start(out=out, in_=in_, **kw), r, w, dma=True)

    def mm(self, out, lhsT, rhs, start=True, stop=True, r=(), w=(), **kw):
        return self.op("pe", lambda e: e.matmul(out, lhsT, rhs, start=start, stop=stop, **kw), r, w)

    def tr(self, out, in_, ident, r=(), w=()):
        return self.op("pe", lambda e: e.transpose(out, in_, ident), r, w)

    def act(self, out, in_, func, r=(), w=(), **kw):
        return self.op("act", lambda e: e.activation(out, in_, func, **kw), r, w)

    def v(self, eng, name, *args, r=(), w=(), **kw):
        return self.op(eng, lambda e: getattr(e, name)(*args, **kw), r, w)

    def emit(self, final_waits=()):
        nc = self.nc
        for e in self.ops:
            for fn, waits, sig, dma in self.ops[e]:
                if sig is not None:
                    self._sem(sig[0])
                for s, v in waits:
                    self._sem(s)
        with nc.Block() as block:
            def body(ename, extra=None):
                def f(eng):
                    for fn, waits, sig, dma in self.ops[ename]:
                        for s, v in waits:
                            eng.wait_ge(self.sems[s], v)
                        if fn is not None:
                            fn(eng).then_inc(self.sems[sig[0]], 16 if dma else 1)
                    for s, v in (extra or ()):
                        eng.wait_ge(self.sems[s], v)
                return f
            block.tensor(body("pe"))
            block.scalar(body("act"))
            block.vector(body("dve"))
            block.gpsimd(body("pool"))
            block.sync(body("sp", list(final_waits)))


class _Stop(Exception):
    pass


STOP = 0


def build(S):
    def stage(n):
        if STOP == n:
            raise _Stop()
    nc = bass.Bass("TRN2", target_bir_lowering=False)
    P = Prog(nc)
    NCH = S // 64
    T = 2 * S
    TT = min(512, T)
    NSUB = TT // 128
    NTT = T // TT
    L = DEPTH

    def din(name, shape, dt=F32):
        return P.dram(name, shape, dt, kind="ExternalInput")

    x_in = din("x_in", [T, D])
    pT = din("pT", [L, 256, T])
    prm = {}
    for name, shape in [("ln_in_w", [1, D]), ("ln_in_b", [1, D]), ("w_in", [L, D, NIN]), ("rwkv_mu", [L, 1024]),
                        ("rwkv_w0", [L, 256]), ("rwkv_w2", [L, 64, 256]), ("rwkv_a0", [L, 256]), ("rwkv_a2", [L, 64, 256]),
                        ("rwkv_g2", [L, 128, 256]), ("rwkv_k_k", [L, 256]), ("rwkv_k_a", [L, 256]), ("rwkv_r_k", [L, 256]),
                        ("rwkv_ln_w", [L, 256]), ("rwkv_ln_b", [L, 256]), ("gla_w2", [L, 16, 128]), ("gla_b", [L, 128]),
                        ("gla_norm_w", [L, 256]), ("hgrn_lb_logits", [L, 256]), ("hgrn_norm_w", [L, 256]),
                        ("w_branch", [L, 4, 256, D]), ("w_mix_out", [L, D, D]), ("ln_mix_w", [L, D]), ("ln_mix_b", [L, D]),
                        ("w_ffn_up", [L, D, 2 * DFF]), ("ffn_convT", [L, 128, 22, 3]), ("w_ffn_down", [L, DFF, D]),
                        ("w_ple_gate", [L, D, D]), ("w_ple_proj", [L, 256, D]), ("ln_ffn_w", [L, D]), ("ln_ffn_b", [L, D]),
                        ("c_cos", [T, 16]), ("c_sin", [T, 16]), ("c_maskI", [128, 512]), ("c_maskS", [128, 512]),
                        ("c_maskST", [128, 512]), ("c_bones", [128, 128]), ("c_bsel", [128, 2]), ("c_hmask", [128, 2, 512]),
                        ("c_ident", [128, 128]), ("c_retq", [128, 256]), ("c_retk", [128, 256]), ("c_retkh", [128, 256]),
                        ("c_retdec", [128, 4])]:
        prm[name] = din(name, shape)
    out_d = P.dram("out", [T, D], F32, kind="ExternalOutput")
    xres = P.dram("xres", [T, D])
    zmix = P.dram("zmix", [T, NMIX])
    oTd = P.dram("oTd", [1024, T], BF16)

    xT = None
    xTd = P.dram("xTd", [128, 8 * T], BF16)

    def xT_alloc(load):
        nonlocal xT
        xT = P.sb("xT", [128, 8, T], BF16)
        if load:
            for k in range(8):
                P.dma(xT[:, k, :], xTd.ap()[:, k * T:(k + 1) * T], r=[xTd], w=[xT])

    def xT_spill():
        for k in range(8):
            P.dma(xTd.ap()[:, k * T:(k + 1) * T], xT[:, k, :], r=[xT], w=[xTd])
    ident = P.sb("ident", [128, 128])
    maskI = P.sb("maskI", [128, 512])
    maskS = P.sb("maskS", [128, 512])
    maskST = P.sb("maskST", [128, 512])
    bones = P.sb("bones", [128, 128])
    bsel = P.sb("bsel", [128, 2])
    hmask = P.sb("hmask", [128, 2, 512])
    retq = P.sb("retq", [128, 256])
    retk = P.sb("retk", [128, 256])
    retkh = P.sb("retkh", [128, 256])
    retdec = P.sb("retdec", [128, 4])
    cosT = P.sb("cosT", [128, NCH, 16])
    sinT = P.sb("sinT", [128, NCH, 16])
    P.pool("ps", 8, [128, 512], F32, "ps")
    _pst = P.rot["ps"][0]
    for i_, tg in enumerate(["ps_rw", "ps_ret", "ps_gla", "ps_hg"]):
        P.rot[tg] = [[_pst[2 * i_], _pst[2 * i_ + 1]], 0]
    cur_ps = ["ps"]

    def PS():
        return P.get(cur_ps[0])
    lnw = lnb = arena = None

    def ln_alloc(nb=1):
        nonlocal lnw, lnb
        lnw = P.sb("lnw", [128, D])
        lnb = P.sb("lnb", [128, D])
        P.pool("xt", nb, [128, D])
        P.pool("xo", nb, [128, D])
        P.pool("xb", nb, [128, D])
        P.pool("st6", 2, [128, 2, 6])
        P.pool("mv", 2, [128, 2])
        P.pool("rs", 2, [128, 1])

    cst = [ident, maskI, maskS, maskST, bones, bsel, hmask, retq, retk, retkh, retdec]
    for t, n in zip(cst, ["c_ident", "c_maskI", "c_maskS", "c_maskST", "c_bones", "c_bsel", "c_hmask", "c_retq", "c_retk",
                          "c_retkh", "c_retdec"]):
        P.dma(t.h.ap(), prm[n].ap(), w=[t])
    P.dma(cosT[:, :, :], prm["c_cos"].ap().rearrange("(c p) i -> p c i", p=128), w=[cosT])
    P.dma(sinT[:, :, :], prm["c_sin"].ap().rearrange("(c p) i -> p c i", p=128), w=[sinT])

    def bcast_load(dst, src_row_ap, n):
        P.dma(dst, src_row_ap.partition_broadcast(128), w=[n])

    def layer_norm(xt, tok0, dst_dram, eps=1e-5):
        st6 = P.get("st6")
        mv = P.get("mv")
        rs = P.get("rs")
        for hh in range(2):
            P.v("dve", "bn_stats", st6[:, hh, :], xt[:, hh * 512:(hh + 1) * 512], r=[xt], w=[st6])
        P.v("dve", "bn_aggr", mv[:, :], st6[:, :, :].rearrange("p a b -> p (a b)"), r=[st6], w=[mv])
        P.act(rs[:, :], mv[:, 1:2], AF.Sqrt, r=[mv], w=[rs], bias=epst[:, 0:1], scale=1.0)
        P.v("dve", "reciprocal", rs[:, :], rs[:, :], r=[rs], w=[rs])
        xo = P.get("xo")
        P.v("dve", "tensor_scalar", xo[:, :], xt[:, :], mv[:, 0:1], rs[:, 0:1], ALU.subtract, ALU.mult, r=[xt, mv, rs], w=[xo])
        P.v("dve", "tensor_tensor", xo[:, :], xo[:, :], lnw[:, :], ALU.mult, r=[xo, lnw], w=[xo])
        P.v("dve", "tensor_tensor", xo[:, :], xo[:, :], lnb[:, :], ALU.add, r=[xo, lnb], w=[xo])
        sig = P.dma(dst_dram.ap()[tok0:tok0 + 128, :], xo[:, :], r=[xo], w=[dst_dram.k(tok0)])
        for g in range(2):
            ps = PS()
            for kk in range(4):
                k8 = g * 4 + kk
                P.tr(ps[:, kk * 128:(kk + 1) * 128], xo[:, k8 * 128:(k8 + 1) * 128], ident[:, :], r=[xo, ident], w=[ps])
            P.act(xT[:, g * 4:(g + 1) * 4, tok0:tok0 + 128], ps[:, :].rearrange("p (k t) -> p k t", k=4), AF.Copy,
                  r=[ps], w=[xT.k(tok0 // 128)])
        return sig

    epst = P.sb("epst", [128, 4])
    P.v("dve", "memset", epst[:, 0:1], 1e-5, w=[epst])
    P.v("dve", "memset", epst[:, 1:2], 1e-6, w=[epst])
    P.v("dve", "memset", epst[:, 2:3], 64e-5, w=[epst])
    P.v("dve", "memset", epst[:, 3:4], 1e-12, w=[epst])

    P.push()
    xT_alloc(False)
    P.push()
    ln_alloc(3)
    bcast_load(lnw[:, :], prm["ln_in_w"].ap()[0:1, :], lnw)
    bcast_load(lnb[:, :], prm["ln_in_b"].ap()[0:1, :], lnb)
    for c in range(T // 128):
        xt = P.get("xt")
        P.dma(xt[:, :], x_in.ap()[c * 128:(c + 1) * 128, :], w=[xt])
        layer_norm(xt, c * 128, xres)
    P.pop()

    zp = mub = w2s = g2s = gw2s = None
    pb = {}
    ST = {}
    STb = {}
    wk = {}

    def mixer_alloc():
        nonlocal zp, mub, w2s, g2s, gw2s
        P.pool("zt", 2, [128, NMIX])
        P.pool("zp", 2, [128, 1024])
        mub = P.sb("mub", [128, 1024])
        for n in ["w0", "a0", "kk", "ka", "rk", "lw_", "lb_", "glab", "glanw", "hlb", "hnw"]:
            pb[n] = P.sb("pb_" + n, [128, 256])
        w2s = P.sb("w2s", [128, 256], BF16)
        g2s = P.sb("g2s", [128, 256], BF16)
        gw2s = P.sb("gw2s", [16, 128], BF16)
        for m in ["ret", "rwkv", "gla", "hgrn"]:
            ST[m] = P.sb("ST_" + m, [128, 2, 2, 256])
            STb[m] = P.sb("STb_" + m, [128, 2, 2, 256], BF16)

    def W(name, shape=(128, 256), dt=F32):
        if name not in wk:
            wk[name] = P.sb("wk_" + name, list(shape), dt)
        return wk[name]


    def tt(out, a, b, op, r, w, eng="dve"):
        P.v(eng, "tensor_tensor", out, a, b, op, r=r, w=w)

    def h3(ap, h=4):
        return ap.rearrange("p (h d) -> p h d", h=h)

    def bc(ap4, n=64):
        return ap4.unsqueeze(2).to_broadcast([128, ap4.shape[1], n])

    def to_fm(src, name, dt=BF16):
        ps = PS()
        for kt in range(2):
            P.tr(ps[:, kt * 128:(kt + 1) * 128], src[:, kt * 128:(kt + 1) * 128], ident[:, :], r=[src, ident], w=[ps])
        dst = W(name, (128, 2, 128), dt)
        P.act(dst[:, :, :], ps[:, 0:256].rearrange("p (k t) -> p k t", k=2), AF.Copy, r=[ps], w=[dst])
        return dst

    def scores(lT, rT, mask, name, dt=BF16):
        pss = [PS(), PS()]
        for h in range(4):
            rows = slice(64 * (h % 2), 64 * (h % 2) + 64)
            ps = pss[h % 2]
            P.mm(ps[:, (h // 2) * 128:(h // 2 + 1) * 128], lT[rows, h // 2, :], rT[rows, h // 2, :], r=[lT, rT], w=[ps])
        dst = W(name, (128, 512), dt)
        d4 = dst[:, :].rearrange("p (g e t) -> p g e t", g=2, e=2)
        m4 = mask[:, :].rearrange("p (g e t) -> p g e t", g=2, e=2)
        for e in range(2):
            tt(d4[:, :, e, :], pss[e][:, 0:256].rearrange("p (g t) -> p g t", g=2), m4[:, :, e, :], ALU.mult,
               r=[pss[e], mask], w=[dst])
        return dst

    def cross(ps, xFM, stb, first=True):
        for b in range(2):
            for kt in range(2):
                P.mm(ps[64 * b:64 * b + 64, 0:256], xFM[:, kt, 64 * b:64 * b + 64], stb[:, kt, b, :],
                     start=(kt == 0), stop=False, r=[xFM, stb], w=[ps], skip_group_check=True)

    def state_update(m, pairs, decay):
        st, stb = ST[m], STb[m]
        for kt in range(2):
            psb = [PS(), PS()]
            tmp = W(m + "_sutmp", (128, 512))
            for b in range(2):
                ps = psb[b]
                for i, (lh, rh) in enumerate(pairs):
                    P.mm(ps[:, 0:256], lh[64 * b:64 * b + 64, kt * 128:(kt + 1) * 128], rh[64 * b:64 * b + 64, 0:256],
                         start=(i == 0), stop=(i == len(pairs) - 1), r=[lh, rh], w=[ps], skip_group_check=True)
                tt(tmp[:, b * 256:(b + 1) * 256], ps[:, 0:256], hmask[:, kt, 0:256], ALU.mult, r=[ps, hmask], w=[tmp])
            for b in range(2):
                P.v("dve", "scalar_tensor_tensor", st[:, kt, b, :], st[:, kt, b, :], decay(kt, b), tmp[:, b * 256:(b + 1) * 256],
                    ALU.mult, ALU.add, r=[st, tmp] + decay.r, w=[st])
        P.act(stb[:, :, :, :].rearrange("p a b c -> p (a b c)"), st[:, :, :, :].rearrange("p a b c -> p (a b c)"), AF.Copy,
              r=[st], w=[stb])

    class Dec:
        def __init__(self, fn, r):
            self.fn, self.r = fn, r

        def __call__(self, kt, b):
            return self.fn(kt, b)

    def rstd_of(sumsq, eps_col, name):
        t = W(name, (128, 4))
        P.act(t[:, :], sumsq[:, :], AF.Sqrt, r=[sumsq, epst], w=[t], bias=epst[:, eps_col:eps_col + 1], scale=1.0 / 64)
        P.v("dve", "reciprocal", t[:, :], t[:, :], r=[t], w=[t])
        return t

    def head_sumsq(src, name):
        sq = W(name + "_sq")
        P.act(sq[:, :], src[:, :], AF.Square, r=[src], w=[sq])
        s = W(name + "_s", (128, 4))
        P.v("dve", "tensor_reduce", s[:, :], h3(sq[:, :]), AX.X, ALU.add, r=[sq], w=[s])
        return s

    def cumsum_decay(la, name):
        ps = PS()
        P.mm(ps[:, 0:256], maskI[:, 0:128], la[:, :], r=[maskI, la], w=[ps])
        P.mm(ps[:, 256:512], bones[:, :], la[:, :], r=[bones, la], w=[ps])
        lac = W(name + "_lac", (128, 512))
        P.v("dve", "tensor_copy", lac[:, :], ps[:, :], r=[ps], w=[lac])
        ps2 = PS()
        for kt in range(2):
            P.mm(ps2[:, kt * 2:kt * 2 + 2], la[:, kt * 128:(kt + 1) * 128], bsel[:, :], r=[la, bsel], w=[ps2])
        dec = W(name + "_dec", (128, 4))
        P.act(dec[:, :], ps2[:, 0:4], AF.Exp, r=[ps2], w=[dec])
        return lac, dec

    def gla_like(m, zt, q_ap, k_ap, v_ap, la, qscale, c):
        yield
        lac, dec = cumsum_decay(la, m)
        e1 = W(m + "_e1")
        yield
        P.act(e1[:, :], lac[:, 0:256], AF.Exp, r=[lac], w=[e1])
        qt_ = W(m + "_qt")
        yield
        P.v("dve", "scalar_tensor_tensor", qt_[:, :], q_ap[0], qscale, e1[:, :], ALU.mult, ALU.mult, r=[q_ap[1], e1], w=[qt_])
        e2 = W(m + "_e2")
        yield
        P.act(e2[:, :], lac[:, 0:256], AF.Exp, r=[lac], w=[e2], scale=-1.0)
        kt_ = W(m + "_kt")
        yield
        tt(kt_[:, :], k_ap[0], e2[:, :], ALU.mult, r=[k_ap[1], e2], w=[kt_])
        d3 = W(m + "_d3")
        yield
        tt(d3[:, :], lac[:, 256:512], lac[:, 0:256], ALU.subtract, r=[lac], w=[d3])
        yield
        P.act(d3[:, :], d3[:, :], AF.Exp, r=[d3], w=[d3])
        kh = W(m + "_kh", (128, 256), BF16)
        yield
        tt(kh[:, :], k_ap[0], d3[:, :], ALU.mult, r=[k_ap[1], d3], w=[kh])
        vb = W(m + "_vb", (128, 256), BF16)
        yield
        P.act(vb[:, :], v_ap[0], AF.Copy, r=[v_ap[1]], w=[vb])
        o = yield from la_core(m, qt_, kt_, kh, vb, Dec(lambda kt, b: dec[:, kt * 2 + b:kt * 2 + b + 1], [dec]))
        return o

    def la_core(m, qt_, kt_, kh, vb, decay):
        yield
        qT = to_fm(qt_, m + "_qT")
        yield
        kT = to_fm(kt_, m + "_kT")
        yield
        A = scores(kT, qT, maskI, m + "_A")
        ps = PS()
        yield
        cross(ps, qT, STb[m])
        for h in range(4):
            yield
            P.mm(ps[:, h * 64:(h + 1) * 64], A[:, h * 128:(h + 1) * 128], vb[:, h * 64:(h + 1) * 64], start=False, stop=(h == 3),
                 r=[A, vb], w=[ps], skip_group_check=True)
        o = W(m + "_o")
        yield
        P.v("dve", "tensor_copy", o[:, :], ps[:, 0:256], r=[ps], w=[o])
        yield
        state_update(m, [(kh, vb)], decay)
        return o

    def finish_branch(n, ob, c):
        oT = to_fm(ob, "oT%d" % n)
        P.dma(oTd.ap()[n * 256:(n + 1) * 256, c * 128:(c + 1) * 128].rearrange("(k p) t -> p k t", p=128), oT[:, :, :],
              r=[oT], w=[oTd.k((n, c))])

    final = []
    try:
        stage(1)
        for l in range(L):
            P.push()
            P.pool("wab", 2, [128, 8, 512], BF16)
            P.pool("zst", 4, [128, 512])
            nblk = (NMIX + 511) // 512
            for cb in range(nblk):
                c0, c1 = cb * 512, min(NMIX, cb * 512 + 512)
                wab = P.get("wab")
                P.dma(wab[:, :, 0:c1 - c0], prm["w_in"].ap()[l, :, c0:c1].rearrange("(k p) n -> p k n", p=128), w=[wab], q="pool")
                for c in range(NCH):
                    tok0 = c * 128
                    ps = PS()
                    for k in range(8):
                        P.mm(ps[:, 0:c1 - c0], xT[:, k, tok0:tok0 + 128], wab[:, k, 0:c1 - c0], start=(k == 0), stop=(k == 7),
                             r=[xT.k(c), wab], w=[ps])
                    zst = P.get("zst")
                    if c % 2 == 0:
                        P.act(zst[:, 0:c1 - c0], ps[:, 0:c1 - c0], AF.Copy, r=[ps], w=[zst])
                    else:
                        P.v("dve", "tensor_copy", zst[:, 0:c1 - c0], ps[:, 0:c1 - c0], r=[ps], w=[zst])
                    P.dma(zmix.ap()[tok0:tok0 + 128, c0:c1], zst[:, 0:c1 - c0], r=[zst], w=[zmix.k(c)])
            P.pop()
            xT_spill()
            P.pop()

            stage(2)
            P.push()
            wk = {}
            mixer_alloc()
            bcast_load(mub[:, :], prm["rwkv_mu"].ap()[l:l + 1, :], mub)
            for n, src in [("w0", "rwkv_w0"), ("a0", "rwkv_a0"), ("kk", "rwkv_k_k"), ("ka", "rwkv_k_a"), ("rk", "rwkv_r_k"),
                           ("lw_", "rwkv_ln_w"), ("lb_", "rwkv_ln_b"), ("glanw", "gla_norm_w"),
                           ("hnw", "hgrn_norm_w")]:
                bcast_load(pb[n][:, :], prm[src].ap()[l:l + 1, :], pb[n])
            bcast_load(pb["glab"][:, 0:128], prm["gla_b"].ap()[l:l + 1, :], pb["glab"])
            if l == 0:
                P.v("dve", "memset", pb["hlb"][:, :], 0.0, w=[pb["hlb"]])
            else:
                hl0 = W("hl0")
                bcast_load(hl0[:, :], prm["hgrn_lb_logits"].ap()[0:1, :], hl0)
                bcast_load(pb["hlb"][:, :], prm["hgrn_lb_logits"].ap()[1:2, :], pb["hlb"])
                tt(pb["hlb"][:, :], pb["hlb"][:, :], hl0[:, :], ALU.subtract, r=[pb["hlb"], hl0], w=[pb["hlb"]])
                P.act(pb["hlb"][:, :], pb["hlb"][:, :], AF.Sigmoid, r=[pb["hlb"]], w=[pb["hlb"]])
            P.dma(w2s[0:64, :], prm["rwkv_w2"].ap()[l], w=[w2s], q="pool")
            P.dma(w2s[64:128, :], prm["rwkv_a2"].ap()[l], w=[w2s], q="pool")
            P.dma(g2s[:, :], prm["rwkv_g2"].ap()[l], w=[g2s], q="pool")
            P.dma(gw2s[:, :], prm["gla_w2"].ap()[l], w=[gw2s], q="pool")
            for m in ST:
                P.v("dve", "memset", ST[m][:, :, :, :].rearrange("p a b c -> p (a b c)"), 0.0, w=[ST[m]])
                P.v("dve", "memset", STb[m][:, :, :, :].rearrange("p a b c -> p (a b c)"), 0.0, w=[STb[m]])

            for c in range(NCH):
                tok0 = c * 128
                zt = P.get("zt")
                zp = P.get("zp")
                P.dma(zt[:, :], zmix.ap()[tok0:tok0 + 128, :], r=[zmix.k(c)], w=[zt])
                P.dma(zp[1:128, :], zmix.ap()[tok0:tok0 + 127, OFF_RWKV:OFF_RWKV + 1024], r=[zmix.k(c)], w=[zp])
                if c > 0:
                    P.dma(zp[0:1, :], zmix.ap()[tok0 - 65:tok0 - 64, OFF_RWKV:OFF_RWKV + 1024], r=[zmix.k(c - 1)], w=[zp])
                    P.dma(zp[64:65, :], zmix.ap()[tok0 - 1:tok0, OFF_RWKV:OFF_RWKV + 1024], r=[zmix.k(c - 1)], w=[zp])
                else:
                    P.v("dve", "memset", zp[0:1, :], 0.0, w=[zp])
                    P.v("dve", "memset", zp[64:65, :], 0.0, w=[zp])

                def ret_gen():
                    qr = W("ret_qr")
                    kr = W("ret_kr")
                    if c == 0:
                        yield
                        P.v("dve", "memset", qr[:, :], 0.0, w=[qr])
                        yield
                        P.v("dve", "memset", kr[:, :], 0.0, w=[kr])
                    cs = cosT[:, c, :].unsqueeze(1).to_broadcast([128, 4, 16])
                    sn = sinT[:, c, :].unsqueeze(1).to_broadcast([128, 4, 16])
                    for src_off, dstt in [(OFF_RET, qr), (OFF_RET + 128, kr)]:
                        src = zt[:, src_off:src_off + 128].rearrange("p (h i two) -> p h i two", h=4, two=2)
                        x1, x2 = src[:, :, :, 0], src[:, :, :, 1]
                        dv = dstt[:, :].rearrange("p (h i two) -> p h i two", h=4, two=2)
                        o1, o2 = dv[:, :, 0:16, 0], dv[:, :, 0:16, 1]
                        t1, t2 = W("rot_t1", (128, 4, 16)), W("rot_t2", (128, 4, 16))
                        yield
                        tt(t1[:, :, :], x1, cs, ALU.mult, r=[zt, cosT], w=[t1])
                        yield
                        tt(t2[:, :, :], x2, sn, ALU.mult, r=[zt, sinT], w=[t2])
                        yield
                        tt(o1, t1[:, :, :], t2[:, :, :], ALU.subtract, r=[t1, t2], w=[dstt])
                        t3, t4 = W("rot_t3", (128, 4, 16)), W("rot_t4", (128, 4, 16))
                        yield
                        tt(t3[:, :, :], x1, sn, ALU.mult, r=[zt, sinT], w=[t3])
                        yield
                        tt(t4[:, :, :], x2, cs, ALU.mult, r=[zt, cosT], w=[t4])
                        yield
                        tt(o2, t3[:, :, :], t4[:, :, :], ALU.add, r=[t3, t4], w=[dstt])
                    qt_ = W("ret_qt")
                    yield
                    tt(qt_[:, :], qr[:, :], retq[:, :], ALU.mult, r=[qr, retq], w=[qt_])
                    kt_ = W("ret_kt")
                    yield
                    tt(kt_[:, :], kr[:, :], retk[:, :], ALU.mult, r=[kr, retk], w=[kt_])
                    kh = W("ret_kh", (128, 256), BF16)
                    yield
                    tt(kh[:, :], kr[:, :], retkh[:, :], ALU.mult, r=[kr, retkh], w=[kh])
                    vb = W("ret_vb", (128, 256), BF16)
                    yield
                    P.act(vb[:, :], zt[:, OFF_RET + 256:OFF_RET + 512], AF.Copy, r=[zt], w=[vb])
                    o = yield from la_core("ret", qt_, kt_, kh, vb, Dec(lambda kt, b: retdec[:, kt:kt + 1], [retdec]))
                    sm = W("ret_sm", (128, 4))
                    yield
                    P.v("dve", "tensor_reduce", sm[:, :], h3(o[:, :]), AX.X, ALU.add, r=[o], w=[sm])
                    yield
                    P.v("dve", "tensor_scalar", sm[:, :], sm[:, :], 1.0 / 64, None, ALU.mult, r=[sm], w=[sm])
                    oc = W("ret_oc")
                    yield
                    tt(h3(oc[:, :]), h3(o[:, :]), bc(sm[:, :]), ALU.subtract, r=[o, sm], w=[oc])
                    yield
                    ss = head_sumsq(oc, "ret_ss")
                    yield
                    rs_ = rstd_of(ss, 1, "ret_rs4")
                    on = W("ret_on")
                    yield
                    tt(h3(on[:, :]), h3(oc[:, :]), bc(rs_[:, :]), ALU.mult, r=[oc, rs_], w=[on])
                    sg = W("ret_sg")
                    yield
                    P.act(sg[:, :], zt[:, OFF_RET + 512:OFF_RET + 768], AF.Silu, r=[zt], w=[sg])
                    ob = W("ret_ob")
                    yield
                    tt(ob[:, :], on[:, :], sg[:, :], ALU.mult, r=[on, sg], w=[ob])
                    yield
                    finish_branch(0, ob, c)


                def gla_gen():
                    zg = OFF_GLA
                    glT = W("gla_glT", (16, 128), BF16)
                    psg = PS()
                    yield
                    P.tr(psg[0:16, 0:128], zt[:, zg + 512:zg + 528], ident[:, :], r=[zt, ident], w=[psg])
                    yield
                    P.act(glT[:, :], psg[0:16, 0:128], AF.Copy, r=[psg], w=[glT])
                    psg2 = PS()
                    yield
                    P.mm(psg2[:, 0:128], glT[:, :], gw2s[:, :], r=[glT, gw2s], w=[psg2])
                    ya = W("gla_y", (128, 128))
                    yield
                    tt(ya[:, :], psg2[:, 0:128], pb["glab"][:, 0:128], ALU.add, r=[psg2, pb["glab"]], w=[ya])
                    yield
                    P.act(ya[:, :], ya[:, :], AF.Exp, r=[ya], w=[ya], scale=-1.0)
                    yield
                    P.act(ya[:, :], ya[:, :], AF.Ln, r=[ya], w=[ya], bias=1.0, scale=1.0)
                    la = W("gla_la")
                    qg = W("gla_q")
                    kg = W("gla_k")
                    if c == 0:
                        for t_ in (la, qg, kg):
                            yield
                            P.v("dve", "memset", t_[:, :], 0.0, w=[t_])
                    yield
                    P.v("dve", "tensor_scalar", h3(la[:, :])[:, :, 0:32], h3(ya[:, :], 4), -1.0 / 16, None, ALU.mult, r=[ya], w=[la])
                    yield
                    P.v("dve", "tensor_copy", h3(qg[:, :])[:, :, 0:32], h3(zt[:, zg:zg + 128]), r=[zt], w=[qg])
                    yield
                    P.act(h3(kg[:, :])[:, :, 0:32], h3(zt[:, zg + 128:zg + 256]), AF.Copy, r=[zt], w=[kg])
                    o = yield from gla_like("gla", zt, (qg[:, :], qg), (kg[:, :], kg), (zt[:, zg + 256:zg + 512], zt), la, 32.0 ** -0.5, c)
                    yield
                    ss = head_sumsq(o, "gla_ss")
                    yield
                    rs_ = rstd_of(ss, 1, "gla_rs4")
                    on = W("gla_on")
                    yield
                    tt(h3(on[:, :]), h3(o[:, :]), bc(rs_[:, :]), ALU.mult, r=[o, rs_], w=[on])
                    yield
                    tt(on[:, :], on[:, :], pb["glanw"][:, :], ALU.mult, r=[on, pb["glanw"]], w=[on])
                    sg = W("gla_sg")
                    yield
                    P.act(sg[:, :], zt[:, zg + 528:zg + 784], AF.Silu, r=[zt], w=[sg])
                    ob = W("gla_ob")
                    yield
                    tt(ob[:, :], on[:, :], sg[:, :], ALU.mult, r=[on, sg], w=[ob])
                    yield
                    finish_branch(2, ob, c)


                def hgrn_gen():
                    zh = OFF_HGRN
                    f = W("hg_f")
                    yield
                    P.act(f[:, :], zt[:, zh + 256:zh + 512], AF.Sigmoid, r=[zt], w=[f])
                    tmp = W("hg_tmp")
                    yield
                    tt(tmp[:, :], f[:, :], pb["hlb"][:, :], ALU.mult, r=[f, pb["hlb"]], w=[tmp])
                    yield
                    tt(f[:, :], f[:, :], tmp[:, :], ALU.subtract, r=[f, tmp], w=[f])
                    yield
                    tt(f[:, :], f[:, :], pb["hlb"][:, :], ALU.add, r=[f, pb["hlb"]], w=[f])
                    la = W("hg_la")
                    yield
                    P.act(la[:, :], f[:, :], AF.Ln, r=[f], w=[la])
                    kh_ = W("hg_k")
                    yield
                    P.v("dve", "tensor_scalar", kh_[:, :], f[:, :], -1.0, 1.0, ALU.mult, ALU.add, r=[f], w=[kh_])
                    o = yield from gla_like("hgrn", zt, (zt[:, zh:zh + 256], zt), (kh_[:, :], kh_), (zt[:, zh + 512:zh + 768], zt), la, 1.0, c)
                    yield
                    ss = head_sumsq(o, "hg_ss")
                    yield
                    rs_ = rstd_of(ss, 1, "hg_rs4")
                    on = W("hg_on")
                    yield
                    tt(h3(on[:, :]), h3(o[:, :]), bc(rs_[:, :]), ALU.mult, r=[o, rs_], w=[on])
                    yield
                    tt(on[:, :], on[:, :], pb["hnw"][:, :], ALU.mult, r=[on, pb["hnw"]], w=[on])
                    sg = W("hg_sg")
                    yield
                    P.act(sg[:, :], zt[:, zh + 768:zh + 1024], AF.Silu, r=[zt], w=[sg])
                    ob = W("hg_ob")
                    yield
                    tt(ob[:, :], on[:, :], sg[:, :], ALU.mult, r=[on, sg], w=[ob])
                    yield
                    finish_branch(3, ob, c)


                def rwkv_gen():
                    zr = OFF_RWKV
                    zs = zp
                    yield
                    tt(zs[:, :], zp[:, :], zt[:, zr:zr + 1024], ALU.subtract, r=[zp, zt], w=[zs])
                    yield
                    tt(zs[:, :], zs[:, :], mub[:, :], ALU.mult, r=[zs, mub], w=[zs])
                    yield
                    tt(zs[:, :], zs[:, :], zt[:, zr:zr + 1024], ALU.add, r=[zs, zt], w=[zs])
                    r_, k_, v_ = zs[:, 0:256], zs[:, 256:512], zs[:, 512:768]
                    li = W("rw_li")
                    yield
                    P.act(li[:, 0:64], zs[:, 768:832], AF.Tanh, r=[zs], w=[li])
                    yield
                    P.act(li[:, 64:128], zs[:, 832:896], AF.Copy, r=[zs], w=[li])
                    yield
                    P.act(li[:, 128:256], zs[:, 896:1024], AF.Sigmoid, r=[zs], w=[li])
                    yield
                    liT = to_fm(li, "rw_liT")
                    psl = PS()
                    yield
                    P.mm(psl[:, 0:256], liT[0:64, 0, :], w2s[0:64, :], r=[liT, w2s], w=[psl])
                    lw = W("rw_lw")
                    yield
                    tt(lw[:, :], psl[:, 0:256], pb["w0"][:, :], ALU.add, r=[psl, pb["w0"]], w=[lw])
                    pslb = PS()
                    yield
                    P.mm(pslb[:, 0:256], liT[64:128, 0, :], w2s[64:128, :], r=[liT, w2s], w=[pslb])
                    a_ = W("rw_a")
                    yield
                    tt(a_[:, :], pslb[:, 0:256], pb["a0"][:, :], ALU.add, r=[pslb, pb["a0"]], w=[a_])
                    psl2 = PS()
                    yield
                    P.mm(psl2[:, 0:256], liT[:, 1, :], g2s[:, :], r=[liT, g2s], w=[psl2])
                    g_ = W("rw_g")
                    yield
                    P.act(g_[:, :], psl2[:, 0:256], AF.Copy, r=[psl2], w=[g_])
                    yield
                    P.act(lw[:, :], lw[:, :], AF.Sigmoid, r=[lw], w=[lw])
                    yield
                    P.v("dve", "tensor_scalar", lw[:, :], lw[:, :], -0.6065306597126334, None, ALU.mult, r=[lw], w=[lw])
                    yield
                    P.act(a_[:, :], a_[:, :], AF.Sigmoid, r=[a_], w=[a_])
                    kk = W("rw_kk")
                    yield
                    tt(kk[:, :], k_, pb["kk"][:, :], ALU.mult, r=[zs, pb["kk"]], w=[kk])
                    sq = W("rw_t1")
                    yield
                    P.act(sq[:, :], kk[:, :], AF.Square, r=[kk], w=[sq])
                    s4 = W("rw_s4", (128, 4))
                    yield
                    P.v("dve", "tensor_reduce", s4[:, :], h3(sq[:, :]), AX.X, ALU.add, r=[sq], w=[s4])
                    rn = W("rw_rn", (128, 4))
                    yield
                    P.act(rn[:, :], s4[:, :], AF.Sqrt, r=[s4, epst], w=[rn], bias=epst[:, 3:4], scale=1.0)
                    yield
                    P.v("dve", "reciprocal", rn[:, :], rn[:, :], r=[rn], w=[rn])
                    kkn = W("rw_kkn")
                    yield
                    tt(h3(kkn[:, :]), h3(kk[:, :]), bc(rn[:, :]), ALU.mult, r=[kk, rn], w=[kkn])
                    kp = W("rw_kp")
                    yield
                    P.v("dve", "scalar_tensor_tensor", kp[:, :], a_[:, :], -1.0, pb["ka"][:, :], ALU.add, ALU.mult, r=[a_, pb["ka"]], w=[kp])
                    yield
                    P.v("dve", "scalar_tensor_tensor", kp[:, :], kp[:, :], 1.0, k_, ALU.add, ALU.mult, r=[kp, zs], w=[kp])
                    bt = W("rw_t1")
                    yield
                    tt(bt[:, :], r_, kp[:, :], ALU.mult, r=[zs, kp], w=[bt])
                    yield
                    tt(bt[:, :], bt[:, :], pb["rk"][:, :], ALU.mult, r=[bt, pb["rk"]], w=[bt])
                    b4 = W("rw_b4", (128, 4))
                    yield
                    P.v("dve", "tensor_reduce", b4[:, :], h3(bt[:, :]), AX.X, ALU.add, r=[bt], w=[b4])
                    bon = W("rw_bon")
                    yield
                    tt(h3(bon[:, :]), h3(v_), bc(b4[:, :]), ALU.mult, r=[zs, b4], w=[bon])
                    yield
                    lac, dec = cumsum_decay(lw, "rw")
                    ea = W("rw_e1")
                    yield
                    tt(ea[:, :], lac[:, 0:256], lw[:, :], ALU.subtract, r=[lac, lw], w=[ea])
                    yield
                    P.act(ea[:, :], ea[:, :], AF.Exp, r=[ea], w=[ea])
                    at = W("rw_tl")
                    yield
                    P.v("dve", "scalar_tensor_tensor", at[:, :], kkn[:, :], -1.0, ea[:, :], ALU.mult, ALU.mult, r=[kkn, ea], w=[at])
                    yield
                    aT = to_fm(at, "rw_aT")
                    bp = W("rw_bp")
                    yield
                    tt(bp[:, :], kkn[:, :], a_[:, :], ALU.mult, r=[kkn, a_], w=[bp])
                    eb = W("rw_e2")
                    yield
                    P.act(eb[:, :], lac[:, 0:256], AF.Exp, r=[lac], w=[eb], scale=-1.0)
                    btl = W("rw_tl")
                    yield
                    tt(btl[:, :], bp[:, :], eb[:, :], ALU.mult, r=[bp, eb], w=[btl])
                    yield
                    bT = to_fm(btl, "rw_bT")
                    ktl = W("rw_tl")
                    yield
                    tt(ktl[:, :], kp[:, :], eb[:, :], ALU.mult, r=[kp, eb], w=[ktl])
                    yield
                    kT = to_fm(ktl, "rw_kT")
                    er = W("rw_e1")
                    yield
                    P.act(er[:, :], lac[:, 0:256], AF.Exp, r=[lac], w=[er])
                    rtl = W("rw_tl")
                    yield
                    tt(rtl[:, :], r_, er[:, :], ALU.mult, r=[zs, er], w=[rtl])
                    yield
                    rT = to_fm(rtl, "rw_rT")
                    eh = W("rw_e2")
                    yield
                    tt(eh[:, :], lac[:, 256:512], lac[:, 0:256], ALU.subtract, r=[lac], w=[eh])
                    yield
                    P.act(eh[:, :], eh[:, :], AF.Exp, r=[eh], w=[eh])
                    bh = W("rw_bh", (128, 256), BF16)
                    yield
                    tt(bh[:, :], bp[:, :], eh[:, :], ALU.mult, r=[bp, eh], w=[bh])
                    khh = W("rw_khh", (128, 256), BF16)
                    yield
                    tt(khh[:, :], kp[:, :], eh[:, :], ALU.mult, r=[kp, eh], w=[khh])
                    vb = W("rw_vb", (128, 256), BF16)
                    yield
                    P.act(vb[:, :], v_, AF.Copy, r=[zs], w=[vb])
                    yield
                    Lm = scores(aT, bT, maskST, "rw_P", F32)
                    yield
                    LmT = scores(bT, aT, maskS, "rw_PT", F32)
                    yield
                    LakT = scores(kT, aT, maskS, "rw_LakT")
                    yield
                    MrbT = scores(bT, rT, maskI, "rw_MrbT")
                    yield
                    MrkT = scores(kT, rT, maskI, "rw_MrkT")
                    ps = PS()
                    yield
                    cross(ps, aT, STb["rwkv"])
                    for h in range(4):
                        yield
                        P.mm(ps[:, h * 64:(h + 1) * 64], LakT[:, h * 128:(h + 1) * 128], vb[:, h * 64:(h + 1) * 64], start=False, stop=(h == 3),
                             r=[LakT, vb], w=[ps], skip_group_check=True)
                    X = W("rw_X0")
                    yield
                    P.v("dve", "tensor_copy", X[:, :], ps[:, 0:256], r=[ps], w=[X])
                    Pm, PmT = Lm, LmT
                    for lev in range(6):
                        if lev < 5:
                            psp = PS()
                            pspt = PS()
                            for h in range(4):
                                hs = slice(h * 128, (h + 1) * 128)
                                yield
                                P.mm(psp[:, hs], PmT[:, hs], Pm[:, hs], r=[PmT, Pm], w=[psp])
                                yield
                                P.mm(pspt[:, hs], Pm[:, hs], PmT[:, hs], r=[PmT, Pm], w=[pspt])
                        psx = PS()
                        for h in range(4):
                            yield
                            P.mm(psx[:, h * 64:(h + 1) * 64], PmT[:, h * 128:(h + 1) * 128], X[:, h * 64:(h + 1) * 64], r=[PmT, X], w=[psx])
                        if lev < 5:
                            sfx = "b" if lev % 2 == 0 else ""
                            Pn = W("rw_P" + sfx, (128, 512))
                            PnT = W("rw_PT" + sfx, (128, 512))
                            yield
                            P.act(Pn[:, :], psp[:, :], AF.Copy, r=[psp], w=[Pn])
                            yield
                            P.v("dve", "tensor_copy", PnT[:, :], pspt[:, :], r=[pspt], w=[PnT])
                        Xn = W("rw_X0")
                        yield
                        tt(Xn[:, :], X[:, :], psx[:, 0:256], ALU.add, r=[X, psx], w=[Xn])
                        X = Xn
                        if lev < 5:
                            Pm, PmT = Pn, PnT
                    ub = W("rw_ub", (128, 256), BF16)
                    yield
                    P.act(ub[:, :], X[:, :], AF.Copy, r=[X], w=[ub])
                    ps = PS()
                    yield
                    cross(ps, rT, STb["rwkv"])
                    for h in range(4):
                        yield
                        P.mm(ps[:, h * 64:(h + 1) * 64], MrbT[:, h * 128:(h + 1) * 128], ub[:, h * 64:(h + 1) * 64], start=False, stop=False,
                             r=[MrbT, ub], w=[ps], skip_group_check=True)
                        yield
                        P.mm(ps[:, h * 64:(h + 1) * 64], MrkT[:, h * 128:(h + 1) * 128], vb[:, h * 64:(h + 1) * 64], start=False, stop=(h == 3),
                             r=[MrkT, vb], w=[ps], skip_group_check=True)
                    y = W("rw_y")
                    yield
                    P.v("dve", "tensor_copy", y[:, :], ps[:, 0:256], r=[ps], w=[y])
                    yield
                    state_update("rwkv", [(bh, ub), (khh, vb)], Dec(lambda kt, b: dec[:, kt * 2 + b:kt * 2 + b + 1], [dec]))
                    sm = W("rw_sm", (128, 4))
                    yield
                    P.v("dve", "tensor_reduce", sm[:, :], h3(y[:, :]), AX.X, ALU.add, r=[y], w=[sm])
                    yield
                    P.v("dve", "tensor_scalar", sm[:, :], sm[:, :], 1.0 / 64, None, ALU.mult, r=[sm], w=[sm])
                    yc = W("rw_oc")
                    yield
                    tt(h3(yc[:, :]), h3(y[:, :]), bc(sm[:, :]), ALU.subtract, r=[y, sm], w=[yc])
                    yield
                    ss = head_sumsq(yc, "rw_ss")
                    yield
                    rs_ = rstd_of(ss, 2, "rw_rs4")
                    yn = W("rw_on")
                    yield
                    tt(h3(yn[:, :]), h3(yc[:, :]), bc(rs_[:, :]), ALU.mult, r=[yc, rs_], w=[yn])
                    yield
                    tt(yn[:, :], yn[:, :], pb["lw_"][:, :], ALU.mult, r=[yn, pb["lw_"]], w=[yn])
                    yield
                    tt(yn[:, :], yn[:, :], pb["lb_"][:, :], ALU.add, r=[yn, pb["lb_"]], w=[yn])
                    yield
                    tt(yn[:, :], yn[:, :], bon[:, :], ALU.add, r=[yn, bon], w=[yn])
                    ob = W("rw_ob")
                    yield
                    tt(ob[:, :], yn[:, :], g_[:, :], ALU.mult, r=[yn, g_], w=[ob])
                    yield
                    finish_branch(1, ob, c)

                rw = rwkv_gen()
                gens = [(rw, "ps_rw"), (ret_gen(), "ps_ret"), (gla_gen(), "ps_gla"), (hgrn_gen(), "ps_hg")]
                for g, tg in [gens[1], gens[2], gens[3], gens[0]]:
                    cur_ps[0] = "ps"
                    for _ in g:
                        pass
                cur_ps[0] = "ps"

            stage(6)
            P.pop()
            P.push()
            xT_alloc(True)
            P.push()
            ln_alloc(2)
            bcast_load(lnw[:, :], prm["ln_mix_w"].ap()[l:l + 1, :], lnw)
            bcast_load(lnb[:, :], prm["ln_mix_b"].ap()[l:l + 1, :], lnb)
            arena = P.sb("wo", [128, 8, D], BF16)
            wo = arena
            P.dma(wo[:, :, :], prm["w_mix_out"].ap()[l].rearrange("(k p) n -> p k n", p=128), w=[arena], q="pool")
            if True:
                P.pool("oTt", 2, [128, 8, TT], BF16)
                P.pool("wg", 6, [128, 8, 128], BF16)
                P.pool("wbk", 6, [128, 2, 128], BF16)
                P.pool("sgc", 2, [128, TT])
                P.pool("acc", 2, [128, TT])
                P.pool("tmpc", 2, [128, TT])
                P.pool("mT", 2, [128, 8, TT], BF16)
            for ti in range(NTT):
                t0 = ti * TT
                oTt = P.get("oTt")
                P.dma(oTt[:, :, :], oTd.ap()[:, t0:t0 + TT].rearrange("(k p) t -> p k t", p=128),
                      r=[oTd.k((n, t0 // 128 + s)) for n in range(4) for s in range(NSUB)], w=[oTt])
                mT = P.get("mT")
                for j in range(8):
                    acc = P.get("acc")
                    for n in range(4):
                        wg = P.get("wg")
                        gc0 = OFF_GATE + n * D + j * 128
                        P.dma(wg[:, :, :], prm["w_in"].ap()[l, :, gc0:gc0 + 128].rearrange("(k p) n -> p k n", p=128), w=[wg], q="pool")
                        wbk = P.get("wbk")
                        P.dma(wbk[:, :, :], prm["w_branch"].ap()[l, n, :, j * 128:(j + 1) * 128].rearrange("(k p) n -> p k n", p=128),
                              w=[wbk], q="pool")
                        psg_ = PS()
                        for k in range(8):
                            P.mm(psg_[:, 0:TT], wg[:, k, :], xT[:, k, t0:t0 + TT], start=(k == 0), stop=(k == 7),
                                 r=[wg] + [xT.k(t0 // 128 + s) for s in range(NSUB)], w=[psg_])
                        psp_ = PS()
                        for k in range(2):
                            P.mm(psp_[:, 0:TT], wbk[:, k, :], oTt[:, n * 2 + k, :], start=(k == 0), stop=(k == 1), r=[wbk, oTt], w=[psp_])
                        sgc = P.get("sgc")
                        P.act(sgc[:, :], psg_[:, 0:TT], AF.Sigmoid, r=[psg_], w=[sgc])
                        if n == 0:
                            tt(acc[:, :], sgc[:, :], psp_[:, 0:TT], ALU.mult, r=[sgc, psp_], w=[acc])
                        else:
                            tm_ = P.get("tmpc")
                            tt(tm_[:, :], sgc[:, :], psp_[:, 0:TT], ALU.mult, r=[sgc, psp_], w=[tm_])
                            tt(acc[:, :], acc[:, :], tm_[:, :], ALU.add, r=[acc, tm_], w=[acc])
                    P.act(mT[:, j, :], acc[:, :], AF.Copy, r=[acc], w=[mT])
                for s in range(NSUB):
                    tok0 = t0 + s * 128
                    xb = P.get("xb")
                    P.dma(xb[:, :], xres.ap()[tok0:tok0 + 128, :], r=[xres.k(tok0)], w=[xb])
                    xt = P.get("xt")
                    for hh in range(2):
                        ps = PS()
                        for k in range(8):
                            P.mm(ps[:, :], mT[:, k, s * 128:(s + 1) * 128], wo[:, k, hh * 512:(hh + 1) * 512], start=(k == 0), stop=(k == 7),
                                 r=[mT, arena], w=[ps])
                        P.v("dve", "scalar_tensor_tensor", xt[:, hh * 512:(hh + 1) * 512], xb[:, hh * 512:(hh + 1) * 512], ALPHA, ps[:, :],
                            ALU.mult, ALU.add, r=[xb, ps], w=[xt])
                    layer_norm(xt, tok0, xres)

            stage(7)
            P.pop()
            P.push()
            wk = {}
            ln_alloc()
            bcast_load(lnw[:, :], prm["ln_ffn_w"].ap()[l:l + 1, :], lnw)
            bcast_load(lnb[:, :], prm["ln_ffn_b"].ap()[l:l + 1, :], lnb)
            arena = P.sb("wd", [128, 22, D], BF16)
            wd = arena
            P.dma(wd[:, :, :], prm["w_ffn_down"].ap()[l].rearrange("(k p) n -> p k n", p=128), w=[arena], q="pool")
            if True:
                wk["wpg"] = P.sb("wpg", [128, 8, D], BF16)
                wk["wpp"] = P.sb("wpp", [128, 2, D], BF16)
                wk["cw"] = P.sb("cw", [128, 22, 3])
                wk["hist"] = P.sb("hist", [128, 22, 2, 2])
                P.pool("wu", 2, [128, 8, 128], BF16)
                P.pool("wv", 2, [128, 8, 128], BF16)
                P.pool("ub", 1, [128, 2, 2 + TT // 2])
                P.pool("cv", 1, [128, 2, TT // 2])
                P.pool("cv2", 1, [128, 2, TT // 2])
                P.pool("hT", 1, [128, 22, TT], BF16)
                P.pool("pTt", 1, [128, 2, TT], BF16)
                P.pool("plg", 1, [128, 512])
            wpg, wpp, cw, hist = wk["wpg"], wk["wpp"], wk["cw"], wk["hist"]
            P.dma(wpg[:, :, :], prm["w_ple_gate"].ap()[l].rearrange("(k p) n -> p k n", p=128), w=[wpg], q="pool")
            P.dma(wpp[:, :, :], prm["w_ple_proj"].ap()[l].rearrange("(k p) n -> p k n", p=128), w=[wpp], q="pool")
            P.dma(cw[:, :, :], prm["ffn_convT"].ap()[l], w=[cw])
            P.v("dve", "memset", hist[:, :, :, :].rearrange("p a b c -> p (a b c)"), 0.0, w=[hist])
            NJ = TT // 2
            for ti in range(NTT):
                t0 = ti * TT
                xk = [xT.k(t0 // 128 + s) for s in range(NSUB)]
                hT = P.get("hT")
                pTt = P.get("pTt")
                P.dma(pTt[:, :, :], pT.ap()[l, :, t0:t0 + TT].rearrange("(k p) t -> p k t", p=128), w=[pTt], q="pool")
                for i in range(22):
                    wu = P.get("wu")
                    wv = P.get("wv")
                    P.dma(wu[:, :, :], prm["w_ffn_up"].ap()[l, :, i * 128:(i + 1) * 128].rearrange("(k p) n -> p k n", p=128), w=[wu], q="pool")
                    P.dma(wv[:, :, :], prm["w_ffn_up"].ap()[l, :, DFF + i * 128:DFF + (i + 1) * 128].rearrange("(k p) n -> p k n", p=128),
                          w=[wv], q="pool")
                    psu = PS()
                    psv = PS()
                    for k in range(8):
                        P.mm(psu[:, 0:TT], wu[:, k, :], xT[:, k, t0:t0 + TT], start=(k == 0), stop=(k == 7), r=[wu] + xk, w=[psu])
                    for k in range(8):
                        P.mm(psv[:, 0:TT], wv[:, k, :], xT[:, k, t0:t0 + TT], start=(k == 0), stop=(k == 7), r=[wv] + xk, w=[psv])
                    ub = P.get("ub")
                    P.act(ub[:, :, 2:2 + NJ].rearrange("p b (c j) -> p b c j", j=64),
                          psu[:, 0:TT].rearrange("p (c b j) -> p b c j", b=2, j=64), AF.Copy, r=[psu], w=[ub])
                    P.v("dve", "tensor_copy", ub[:, :, 0:2], hist[:, i, :, :], r=[hist], w=[ub])
                    P.v("dve", "tensor_copy", hist[:, i, :, :], ub[:, :, NJ:NJ + 2], r=[ub], w=[hist])
                    cv = P.get("cv")
                    P.v("dve", "tensor_scalar", cv[:, :, :], ub[:, :, 0:NJ], cw[:, i, 0:1], None, ALU.mult, r=[ub, cw], w=[cv])
                    cv2 = P.get("cv2")
                    P.v("dve", "scalar_tensor_tensor", cv2[:, :, :], ub[:, :, 1:1 + NJ], cw[:, i, 1:2], cv[:, :, :], ALU.mult, ALU.add,
                        r=[ub, cw, cv], w=[cv2])
                    P.v("dve", "scalar_tensor_tensor", cv[:, :, :], ub[:, :, 2:2 + NJ], cw[:, i, 2:3], cv2[:, :, :], ALU.mult, ALU.add,
                        r=[ub, cw, cv2], w=[cv])
                    P.act(cv2[:, :, :], cv[:, :, :], AF.Gelu_apprx_tanh, r=[cv], w=[cv2])
                    tt(hT[:, i, :].rearrange("p (c b j) -> p b c j", b=2, j=64), cv2[:, :, :].rearrange("p b (c j) -> p b c j", j=64),
                       psv[:, 0:TT].rearrange("p (c b j) -> p b c j", b=2, j=64), ALU.mult, r=[cv2, psv], w=[hT])
                for s in range(NSUB):
                    tok0 = t0 + s * 128
                    xb = P.get("xb")
                    lastsig = None
                    P.dma(xb[:, :], xres.ap()[tok0:tok0 + 128, :], r=[xres.k(tok0)], w=[xb])
                    xt = P.get("xt")
                    for hh in range(2):
                        cs_ = slice(hh * 512, (hh + 1) * 512)
                        psg_ = PS()
                        for k in range(8):
                            P.mm(psg_[:, :], xT[:, k, tok0:tok0 + 128], wpg[:, k, cs_], start=(k == 0), stop=(k == 7),
                                 r=[xT.k(tok0 // 128), wpg], w=[psg_])
                        psq = PS()
                        for k in range(2):
                            P.mm(psq[:, :], pTt[:, k, s * 128:(s + 1) * 128], wpp[:, k, cs_], start=(k == 0), stop=(k == 1), r=[pTt, wpp], w=[psq])
                        plg = P.get("plg")
                        P.act(plg[:, :], psg_[:, :], AF.Sigmoid, r=[psg_], w=[plg])
                        tt(plg[:, :], plg[:, :], psq[:, :], ALU.mult, r=[plg, psq], w=[plg])
                        psd = PS()
                        for k in range(22):
                            P.mm(psd[:, :], hT[:, k, s * 128:(s + 1) * 128], wd[:, k, cs_], start=(k == 0), stop=(k == 21), r=[hT, arena], w=[psd])
                        tt(plg[:, :], plg[:, :], psd[:, :], ALU.add, r=[plg, psd], w=[plg])
                        P.v("dve", "scalar_tensor_tensor", xt[:, cs_], xb[:, cs_], ALPHA, plg[:, :], ALU.mult, ALU.add, r=[xb, plg], w=[xt])
                    sig = layer_norm(xt, tok0, out_d if l == L - 1 else xres)
                    if l == L - 1:
                        final.append(sig)
            P.pop()
            if l == L - 1:
                P.pop()
    except _Stop:
        final = [P.dma(out_d.ap()[0:128, 0:128], ident[:, :], r=[ident], w=[out_d.k('dbg')])]
    P.emit(final_waits=final)
    return nc


def _consts(S):
    T = 2 * S
    c = {}
    theta = (1.0 / (10000.0 ** np.linspace(0.0, 1.0, 16))).astype(np.float32)
    pos = np.arange(S, dtype=np.float32)
    ang = (pos[:, None] * theta[None, :]).astype(np.float32)
    cos, sin = np.cos(ang).astype(np.float32), np.sin(ang).astype(np.float32)
    idx = np.arange(S).reshape(S // 64, 1, 64).repeat(2, axis=1).reshape(-1)
    c["c_cos"], c["c_sin"] = cos[idx], sin[idx]
    p = np.arange(128)
    b, j = p // 64, p % 64
    same = b[:, None] == b[None, :]
    mI = (same & (j[:, None] <= j[None, :])).astype(np.float32)
    mS = (same & (j[:, None] < j[None, :])).astype(np.float32)
    c["c_maskI"] = np.tile(mI, (1, 4))
    c["c_maskS"] = np.tile(mS, (1, 4))
    c["c_maskST"] = np.tile(mS.T.copy(), (1, 4))
    c["c_bones"] = same.astype(np.float32)
    c["c_bsel"] = np.stack([(b == 0), (b == 1)], 1).astype(np.float32)
    hm = np.zeros((128, 2, 2, 4, 64), np.float32)
    for kt in range(2):
        for pp in range(128):
            hm[pp, kt, :, 2 * kt + pp // 64, :] = 1.0
    c["c_hmask"] = hm.reshape(128, 2, 512)
    c["c_ident"] = np.eye(128, dtype=np.float32)
    lg = np.log1p(-np.exp2(-5.0 - np.arange(4, dtype=np.float32))).astype(np.float32)
    jj = j.astype(np.float32)
    q = np.exp(lg[None, :] * (jj[:, None] + 1.0))
    k = np.exp(-lg[None, :] * (jj[:, None] + 1.0)) * 32.0 ** -0.5
    kh = np.exp(lg[None, :] * (63.0 - jj[:, None])) * 32.0 ** -0.5
    c["c_retq"] = np.repeat(q, 64, 1).astype(np.float32)
    c["c_retk"] = np.repeat(k, 64, 1).astype(np.float32)
    c["c_retkh"] = np.repeat(kh, 64, 1).astype(np.float32)
    dec = np.exp(lg * 64.0)
    rd = np.zeros((128, 4), np.float32)
    for kt in range(2):
        rd[:, kt] = dec[2 * kt + p // 64]
    c["c_retdec"] = rd
    return {k_: np.ascontiguousarray(v, dtype=np.float32) for k_, v in c.items()}


def prep_inputs(inputs, S, ncores):
    f = lambda a: np.ascontiguousarray(np.asarray(a, dtype=np.float32))
    x, p = f(inputs["x"]), f(inputs["p"])
    L = DEPTH
    shared = {}
    for n in ["w_in", "rwkv_mu", "rwkv_w0", "rwkv_w2", "rwkv_a0", "rwkv_a2", "rwkv_g2", "rwkv_k_k", "rwkv_k_a", "rwkv_ln_w",
              "rwkv_ln_b", "gla_w2", "gla_b", "w_branch", "w_mix_out", "ln_mix_w", "ln_mix_b", "w_ffn_up", "w_ffn_down",
              "w_ple_gate", "w_ple_proj", "ln_ffn_w", "ln_ffn_b"]:
        shared[n] = f(inputs[n])
    shared["ln_in_w"] = f(inputs["ln_in_w"]).reshape(1, D)
    shared["ln_in_b"] = f(inputs["ln_in_b"]).reshape(1, D)
    shared["rwkv_r_k"] = f(inputs["rwkv_r_k"]).reshape(L, 256)
    shared["gla_norm_w"] = np.ascontiguousarray(np.tile(f(inputs["gla_norm_w"]), (1, 4)))
    shared["hgrn_norm_w"] = np.ascontiguousarray(np.tile(f(inputs["hgrn_norm_w"]), (1, 4)))
    shared["hgrn_lb_logits"] = f(inputs["hgrn_lb_logits"])
    cv = f(inputs["ffn_conv"])
    shared["ffn_convT"] = np.ascontiguousarray(cv.reshape(L, 3, 22, 128).transpose(0, 3, 2, 1))
    shared.update(_consts(S))
    maps = []
    for c in range(ncores):
        xs = x[2 * c:2 * c + 2]
        xi = np.ascontiguousarray(xs.reshape(2, S // 64, 64, D).transpose(1, 0, 2, 3).reshape(2 * S, D))
        ps_ = p[:, 2 * c:2 * c + 2]
        pi = ps_.reshape(L, 2, S // 64, 64, 256).transpose(0, 4, 2, 1, 3).reshape(L, 256, 2 * S)
        m = dict(shared)
        m["x_in"] = xi
        m["pT"] = np.ascontiguousarray(pi)
        maps.append(m)
    return maps


def unshuffle(outs, S):
    res = []
    for o in outs:
        res.append(o.reshape(S // 64, 2, 64, D).transpose(1, 0, 2, 3).reshape(2, S, D))
    return np.concatenate(res, 0)


def kernel(**inputs):
    S = 2048
    ncores = 8
    nc = build(S)
    maps = prep_inputs(inputs, S, ncores)
    res = run_bass_kernel_spmd(nc, maps, core_ids=list(range(ncores)))
    return unshuffle([np.asarray(r["out"]) for r in res.results], S).astype(np.float32)
```
